# Optimizing a Trainium2 kernel written in Bass

```python
import math
import jax
import jax.numpy as jnp
from jax import lax
import numpy as np

D_MODEL = 2048
BATCH = 4
SEQ = 8192
DEPTH = 4

N_MIXERS = 3
N_ATTN_LAYERS = (DEPTH + 2) // 3
N_RWKV_LAYERS = (DEPTH + 1) // 3
N_POOL_LAYERS = DEPTH // 3

HEAD_DIM = 64
N_HEADS = D_MODEL // HEAD_DIM
N_KV_HEADS = max(1, N_HEADS // 8)
GQA_GROUP = N_HEADS // N_KV_HEADS
WINDOW = 128
Q_DIM = N_HEADS * HEAD_DIM
KV_DIM = N_KV_HEADS * HEAD_DIM
QKV_DIM = Q_DIM + 2 * KV_DIM

N_BUCKETS = 32
MAX_DISTANCE = 128

RWKV_HEAD = 64
RWKV_HEADS = D_MODEL // RWKV_HEAD
DECAY_LORA = max(32, int(round(1.8 * D_MODEL ** 0.5 / 32)) * 32)
AAA_LORA = max(32, int(round(1.8 * D_MODEL ** 0.5 / 32)) * 32)
GATE_LORA = max(32, int(round(0.6 * D_MODEL ** 0.8 / 32)) * 32)
GN_EPS = 64e-5

POOL_WINDOWS = (2, 4, 8, 16)
POOL_GROUP = D_MODEL // len(POOL_WINDOWS)

N_GROUPS = 4
EXPERTS_PER_GROUP = 8
N_EXPERTS = N_GROUPS * EXPERTS_PER_GROUP
TOP_K = 2
D_EXPERT = D_MODEL // 4
MOE_BLOCK = 256

NORM_EPS = 1e-6

kernel_name = 'hybrid_swa_rwkv7_pool_hmoe'


def _rms_norm(x, gain):
    xf = x.astype(jnp.float32)
    return xf * lax.rsqrt(jnp.mean(xf * xf, axis=-1, keepdims=True) + NORM_EPS) * gain.astype(jnp.float32)


def _ada_rms_norm(x, gain, mod):
    shift, scale, gate = jnp.split(mod, 3, axis=-1)
    h = _rms_norm(x, gain) * (1.0 + scale[:, None, :]) + shift[:, None, :]
    return h.astype(x.dtype), gate[:, None, :]


def _t5_bucket(dist):
    max_exact = N_BUCKETS // 2
    n = jnp.maximum(dist, 0)
    nf = jnp.maximum(n, 1).astype(jnp.float32)
    large = max_exact + (jnp.log(nf / max_exact) / math.log(MAX_DISTANCE / max_exact)
                         * (N_BUCKETS - max_exact)).astype(jnp.int32)
    large = jnp.minimum(large, N_BUCKETS - 1)
    return jnp.where(n < max_exact, n, large)


def _band_bias(rel_bias):
    i = jnp.arange(WINDOW)[:, None]
    j = jnp.arange(2 * WINDOW)[None, :]
    b = rel_bias[_t5_bucket(WINDOW + i - j)]
    return jnp.transpose(b, (2, 0, 1)).reshape(N_KV_HEADS, GQA_GROUP, WINDOW, 2 * WINDOW).astype(jnp.float32)


def _sliding_window_attention(h, bias, w_in, w_o, q_gain, k_gain, sinks):
    B_, S_, _ = h.shape
    nb = S_ // WINDOW
    qkv = h @ w_in
    q = qkv[..., :Q_DIM].reshape(B_, S_, N_KV_HEADS, GQA_GROUP, HEAD_DIM)
    k = qkv[..., Q_DIM:Q_DIM + KV_DIM].reshape(B_, S_, N_KV_HEADS, HEAD_DIM)
    v = qkv[..., Q_DIM + KV_DIM:].reshape(B_, S_, N_KV_HEADS, HEAD_DIM)
    q = _rms_norm(q, q_gain).reshape(B_, nb, WINDOW, N_KV_HEADS, GQA_GROUP, HEAD_DIM)
    k = _rms_norm(k, k_gain)

    def band(t):
        tb = t.astype(jnp.float32).reshape(B_, nb, WINDOW, N_KV_HEADS, HEAD_DIM)
        prev = jnp.pad(tb, ((0, 0), (1, 0), (0, 0), (0, 0), (0, 0)))[:, :-1]
        return jnp.concatenate([prev, tb], axis=2)

    kb, vb = band(k), band(v)
    logits = jnp.einsum('bnqhgd,bnkhd->bnhgqk', q, kb) * (HEAD_DIM ** -0.5) + bias
    i = jnp.arange(WINDOW)[:, None]
    j = jnp.arange(2 * WINDOW)[None, :]
    in_band = (j > i) & (j <= i + WINDOW)
    blk = jnp.arange(nb)[:, None, None]
    valid = in_band[None] & ((blk > 0) | (j >= WINDOW)[None])
    logits = jnp.where(valid[None, :, None, None], logits, -jnp.inf)
    sink = sinks.astype(jnp.float32).reshape(N_KV_HEADS, GQA_GROUP)[None, None, :, :, None]
    m = jnp.maximum(logits.max(axis=-1), sink)
    p = jnp.exp(logits - m[..., None])
    p = p / (p.sum(axis=-1) + jnp.exp(sink - m))[..., None]
    o = jnp.einsum('bnhgqk,bnkhd->bnqhgd', p, vb)
    o = o.reshape(B_, S_, Q_DIM).astype(h.dtype)
    return o @ w_o


def _rwkv7_time_mix(h, mu, w_rkv, w0, w1, w2, a0, a1, a2, g1, g2, k_k, k_a, r_k, lnx_g, lnx_b, w_o):
    B_, S_, D_ = h.shape
    f32 = jnp.float32
    xx = jnp.pad(h, ((0, 0), (1, 0), (0, 0)))[:, :-1] - h
    x_rkv = h[:, :, None, :] + xx[:, :, None, :] * mu[:3]
    rkv = jnp.einsum('bsjd,jde->bsje', x_rkv, w_rkv)
    xw = h + xx * mu[3]
    xa = h + xx * mu[4]
    xg = h + xx * mu[5]
    w_log = -jax.nn.softplus(-(w0 + jnp.tanh(xw @ w1) @ w2).astype(f32)) - 0.5
    a = jax.nn.sigmoid((a0 + (xa @ a1) @ a2).astype(f32))
    g = jax.nn.sigmoid(xg @ g1) @ g2

    def heads(t):
        return t.astype(f32).reshape(B_, S_, RWKV_HEADS, RWKV_HEAD)

    r, k, v = heads(rkv[:, :, 0]), heads(rkv[:, :, 1]), heads(rkv[:, :, 2])
    decay = jnp.exp(-jnp.exp(heads(w_log)))
    a = heads(a)
    kk = k * k_k.astype(f32).reshape(RWKV_HEADS, RWKV_HEAD)
    kk = kk / jnp.maximum(jnp.sqrt(jnp.sum(kk * kk, axis=-1, keepdims=True)), 1e-12)
    k = k * (1.0 + (a - 1.0) * k_a.astype(f32).reshape(RWKV_HEADS, RWKV_HEAD))

    def step(state, inp):
        r_t, w_t, k_t, v_t, a_t, b_t = inp
        sa = jnp.einsum('bhvk,bhk->bhv', state, a_t)
        state = state * w_t[:, :, None, :] + sa[..., None] * b_t[:, :, None, :] + v_t[..., None] * k_t[:, :, None, :]
        return state, jnp.einsum('bhvk,bhk->bhv', state, r_t)

    state0 = jnp.zeros((B_, RWKV_HEADS, RWKV_HEAD, RWKV_HEAD), f32)
    xs = tuple(jnp.moveaxis(t, 1, 0) for t in (r, decay, k, v, -kk, kk * a))
    _, y = lax.scan(step, state0, xs)
    y = jnp.moveaxis(y, 0, 1)
    mean = y.mean(axis=-1, keepdims=True)
    var = jnp.mean((y - mean) ** 2, axis=-1, keepdims=True)
    y = (y - mean) * lax.rsqrt(var + GN_EPS) * lnx_g.astype(f32).reshape(RWKV_HEADS, RWKV_HEAD) \
        + lnx_b.astype(f32).reshape(RWKV_HEADS, RWKV_HEAD)
    y = y + jnp.sum(r * k * r_k.astype(f32), axis=-1, keepdims=True) * v
    y = y.reshape(B_, S_, D_) * g
    return y.astype(h.dtype) @ w_o


def _multiscale_pool(h, w_grp, scale):
    B_, S_, D_ = h.shape
    f32 = jnp.float32
    hf = h.astype(f32)
    csum = jnp.concatenate([jnp.zeros((B_, 1, D_), f32), jnp.cumsum(hf, axis=1)], axis=1)
    counts = jnp.arange(1, S_ + 1, dtype=f32)
    groups = []
    for gi, w in enumerate(POOL_WINDOWS):
        sl = slice(gi * POOL_GROUP, (gi + 1) * POOL_GROUP)
        c_g = csum[:, :, sl]
        lagged = jnp.concatenate([jnp.zeros((B_, w - 1, POOL_GROUP), f32), c_g[:, :S_ - w + 1]], axis=1)
        mean = (c_g[:, 1:] - lagged) / jnp.minimum(counts, float(w))[None, :, None]
        groups.append(mean - hf[:, :, sl])
    pooled = jnp.stack(groups, axis=2)
    mixed = jnp.einsum('bsgc,gce->bsge', pooled, w_grp.astype(f32)).reshape(B_, S_, D_)
    return (mixed * scale.astype(f32)).astype(h.dtype)


def _hier_moe(h, w_grp, b_grp, w_exp, b_exp, w_gate, w_up, w_down):
    B_, S_, D_ = h.shape
    f32 = jnp.float32
    t = h.reshape(-1, D_)
    T = t.shape[0]
    grp_logits = (t @ w_grp).astype(f32) + b_grp.astype(f32)
    grp_prob = jax.nn.softmax(grp_logits, axis=-1)
    grp_idx = jnp.argmax(grp_logits, axis=-1).astype(jnp.int32)
    grp_w = jnp.take_along_axis(grp_prob, grp_idx[:, None], axis=-1)
    exp_logits = ((t @ w_exp).astype(f32) + b_exp.astype(f32)).reshape(T, N_GROUPS, EXPERTS_PER_GROUP)
    exp_logits = jnp.take_along_axis(exp_logits, grp_idx[:, None, None], axis=1)[:, 0]
    top_p, top_i = lax.top_k(jax.nn.softmax(exp_logits, axis=-1), TOP_K)
    weights = grp_w * top_p / jnp.sum(top_p, axis=-1, keepdims=True)
    flat_ids = (grp_idx[:, None] * EXPERTS_PER_GROUP + top_i).reshape(-1).astype(jnp.int32)

    N = T * TOP_K
    order = jnp.argsort(flat_ids).astype(jnp.int32)
    sorted_ids = flat_ids[order]
    counts = jnp.bincount(flat_ids, length=N_EXPERTS).astype(jnp.int32)
    start = jnp.cumsum(counts) - counts
    padded = ((counts + MOE_BLOCK - 1) // MOE_BLOCK) * MOE_BLOCK
    seg_end = jnp.cumsum(padded).astype(jnp.int32)
    pad_start = seg_end - padded
    rank = jnp.arange(N, dtype=jnp.int32) - start[sorted_ids]
    dest_sorted = pad_start[sorted_ids] + rank
    n_blocks = -(-N // MOE_BLOCK) + N_EXPERTS
    P = n_blocks * MOE_BLOCK
    slot_tok = jnp.full((P,), T, jnp.int32).at[dest_sorted].set(order // TOP_K)
    t_pad = jnp.concatenate([t, jnp.zeros((1, D_), t.dtype)], axis=0)
    buf = t_pad[slot_tok].reshape(n_blocks, MOE_BLOCK, D_)
    block_e = jnp.clip(jnp.searchsorted(seg_end, jnp.arange(n_blocks, dtype=jnp.int32) * MOE_BLOCK, side='right'),
                       0, N_EXPERTS - 1)

    def expert_block(args):
        xb, e = args
        hid = jax.nn.silu(xb @ w_gate[e]) * (xb @ w_up[e])
        return hid @ w_down[e]

    out = lax.map(expert_block, (buf, block_e)).reshape(P, D_)
    dest = jnp.zeros((N,), jnp.int32).at[order].set(dest_sorted)
    y = jnp.einsum('tkd,tk->td', out[dest].reshape(T, TOP_K, D_).astype(f32), weights)
    return y.reshape(B_, S_, D_).astype(h.dtype)


def setup_inputs(seed: int = 0) -> dict:
    key = jax.random.key(seed)
    keys = list(jax.random.split(key, 40))
    f32 = jnp.float32
    D = D_MODEL

    def nrm(shape, s):
        return jax.random.normal(keys.pop(), shape, f32) * s

    def near_one(shape):
        return 1.0 + nrm(shape, 0.1)

    NA, NR, NP = N_ATTN_LAYERS, N_RWKV_LAYERS, N_POOL_LAYERS
    inputs = {
        'x': nrm((BATCH, SEQ, D), 1.0),
        'c': nrm((BATCH, D), 1.0),
        'norm_g': near_one((DEPTH, 2, D)),
        'ada_w': nrm((DEPTH, 2, D, 3 * D), 0.5 * D ** -0.5),
        'ada_b': nrm((DEPTH, 2, 3 * D), 0.02),
        'rel_bias': nrm((N_BUCKETS, N_HEADS), 0.5),
        'attn_w_in': nrm((NA, D, QKV_DIM), D ** -0.5),
        'attn_w_o': nrm((NA, Q_DIM, D), Q_DIM ** -0.5),
        'attn_q_gain': near_one((NA, HEAD_DIM)),
        'attn_k_gain': near_one((NA, HEAD_DIM)),
        'attn_sinks': nrm((NA, N_HEADS), 0.5),
        'rw_mu': jax.random.uniform(keys.pop(), (NR, 6, D), f32),
        'rw_w_rkv': nrm((NR, 3, D, D), D ** -0.5),
        'rw_w0': -0.5 + nrm((NR, D), 0.5),
        'rw_w1': nrm((NR, D, DECAY_LORA), D ** -0.5),
        'rw_w2': nrm((NR, DECAY_LORA, D), 0.5 * DECAY_LORA ** -0.5),
        'rw_a0': nrm((NR, D), 0.5),
        'rw_a1': nrm((NR, D, AAA_LORA), D ** -0.5),
        'rw_a2': nrm((NR, AAA_LORA, D), 0.5 * AAA_LORA ** -0.5),
        'rw_g1': nrm((NR, D, GATE_LORA), D ** -0.5),
        'rw_g2': nrm((NR, GATE_LORA, D), GATE_LORA ** -0.5),
        'rw_k_k': near_one((NR, D)),
        'rw_k_a': near_one((NR, D)),
        'rw_r_k': nrm((NR, RWKV_HEADS, RWKV_HEAD), 0.1),
        'rw_lnx_g': near_one((NR, D)),
        'rw_lnx_b': nrm((NR, D), 0.02),
        'rw_w_o': nrm((NR, D, D), D ** -0.5),
        'pool_w': nrm((NP, len(POOL_WINDOWS), POOL_GROUP, POOL_GROUP), POOL_GROUP ** -0.5),
        'pool_scale': near_one((NP, D)),
        'moe_w_grp': nrm((DEPTH, D, N_GROUPS), D ** -0.5),
        'moe_b_grp': nrm((DEPTH, N_GROUPS), 0.01),
        'moe_w_exp': nrm((DEPTH, D, N_EXPERTS), D ** -0.5),
        'moe_b_exp': nrm((DEPTH, N_EXPERTS), 0.01),
        'moe_w_gate': nrm((DEPTH, N_EXPERTS, D, D_EXPERT), D ** -0.5),
        'moe_w_up': nrm((DEPTH, N_EXPERTS, D, D_EXPERT), D ** -0.5),
        'moe_w_down': nrm((DEPTH, N_EXPERTS, D_EXPERT, D), D_EXPERT ** -0.5),
    }
    return inputs


def reference(x, c, norm_g, ada_w, ada_b, rel_bias, attn_w_in, attn_w_o, attn_q_gain, attn_k_gain, attn_sinks,
              rw_mu, rw_w_rkv, rw_w0, rw_w1, rw_w2, rw_a0, rw_a1, rw_a2, rw_g1, rw_g2, rw_k_k, rw_k_a, rw_r_k,
              rw_lnx_g, rw_lnx_b, rw_w_o, pool_w, pool_scale,
              moe_w_grp, moe_b_grp, moe_w_exp, moe_b_exp, moe_w_gate, moe_w_up, moe_w_down):
    c_act = jax.nn.silu(c)
    bias = _band_bias(rel_bias)
    for layer in range(DEPTH):
        kind, idx = layer % N_MIXERS, layer // N_MIXERS
        h, gate = _ada_rms_norm(x, norm_g[layer, 0], c_act @ ada_w[layer, 0] + ada_b[layer, 0])
        if kind == 0:
            y = _sliding_window_attention(h, bias, attn_w_in[idx], attn_w_o[idx], attn_q_gain[idx],
                                          attn_k_gain[idx], attn_sinks[idx])
        elif kind == 1:
            y = _rwkv7_time_mix(h, rw_mu[idx], rw_w_rkv[idx], rw_w0[idx], rw_w1[idx], rw_w2[idx],
                                rw_a0[idx], rw_a1[idx], rw_a2[idx], rw_g1[idx], rw_g2[idx],
                                rw_k_k[idx], rw_k_a[idx], rw_r_k[idx], rw_lnx_g[idx], rw_lnx_b[idx], rw_w_o[idx])
        else:
            y = _multiscale_pool(h, pool_w[idx], pool_scale[idx])
        x = x + gate * y
        h, gate = _ada_rms_norm(x, norm_g[layer, 1], c_act @ ada_w[layer, 1] + ada_b[layer, 1])
        x = x + gate * _hier_moe(h, moe_w_grp[layer], moe_b_grp[layer], moe_w_exp[layer], moe_b_exp[layer],
                                 moe_w_gate[layer], moe_w_up[layer], moe_w_down[layer])
    return x
```

```python
import contextlib
import math
import numpy as np
import concourse.bass as bass
import concourse.mybir as mybir
from concourse.bass_utils import run_bass_kernel_spmd

F32, BF16 = mybir.dt.float32, mybir.dt.bfloat16
AF = mybir.ActivationFunctionType
ALU = mybir.AluOpType
AX = mybir.AxisListType


class Cfg:
    def __init__(self, D=2048, S=8192, DEPTH=4, B=4):
        self.D, self.S, self.DEPTH, self.B = D, S, DEPTH, B
        self.KC = D // 128
        self.H = D // 64
        self.KV = max(1, self.H // 8)
        self.GQ = self.H // self.KV
        self.QKV = D + 2 * self.KV * 64
        self.LW = max(32, int(round(1.8 * D ** 0.5 / 32)) * 32)
        self.LA = self.LW
        self.LG = max(32, int(round(0.6 * D ** 0.8 / 32)) * 32)
        self.DE = D // 4
        self.PG = D // 4
        self.NE, self.NG, self.EPG = 32, 4, 8
        self.NA, self.NR, self.NP = (DEPTH + 2) // 3, (DEPTH + 1) // 3, DEPTH // 3
        self.NT = 512 if S >= 512 else S
        self.NSUB = self.NT // 128


class Buf:
    __slots__ = ("t", "w", "r")

    def __init__(self, t):
        self.t, self.w, self.r = t, {}, {}

    def __getitem__(self, k):
        return self.t[k]


class Sched:
    NDMA = 16

    def __init__(self, nc, es):
        self.nc = nc
        self.eng = dict(pe=nc.tensor, dve=nc.vector, act=nc.scalar, pool=nc.gpsimd, sp=nc.sync)
        self.sem, self.cnt, self.inc = {}, {}, {}
        for n in self.eng:
            self.sem[n] = es.enter_context(nc.semaphore("s_" + n))
            self.cnt[n], self.inc[n] = 0, 1
        for i in range(self.NDMA):
            n = "d%d" % i
            self.sem[n] = es.enter_context(nc.semaphore("s_" + n))
            self.cnt[n], self.inc[n] = 0, 16
        self.known = {e: {} for e in self.eng}
        self.di = 0

    def _waits(self, e, reads, writes):
        deps = {}
        for b in reads:
            for k, v in b.w.items():
                deps[k] = max(deps.get(k, 0), v)
        for b in writes:
            for k, v in b.w.items():
                deps[k] = max(deps.get(k, 0), v)
            for k, v in b.r.items():
                deps[k] = max(deps.get(k, 0), v)
        kn = self.known[e]
        for k, v in deps.items():
            if k == e and e == "pe":
                continue
            if kn.get(k, 0) < v:
                self.eng[e].wait_ge(self.sem[k], v)
                kn[k] = v

    def op(self, e, fn, reads=(), writes=()):
        self._waits(e, reads, writes)
        ins = fn(self.eng[e])
        self.cnt[e] += 1
        v = self.cnt[e]
        ins.then_inc(self.sem[e], 1)
        for b in reads:
            b.r[e] = v
        for b in writes:
            b.w = {e: v}
            b.r = {}

    def dma(self, q, out, in_, reads=(), writes=()):
        self._waits(q, reads, writes)
        n = "d%d" % self.di
        self.di = (self.di + 1) % self.NDMA
        ins = self.eng[q].dma_start(out=out, in_=in_)
        self.cnt[n] += 1
        v = self.cnt[n] * 16
        ins.then_inc(self.sem[n], 16)
        for b in reads:
            b.r[n] = v
        for b in writes:
            b.w = {n: v}
            b.r = {}

    def barrier(self):
        for e in self.eng:
            for k in self.sem:
                v = self.cnt[k] * self.inc[k]
                if v > 0 and k != e and self.known[e].get(k, 0) < v:
                    self.eng[e].wait_ge(self.sem[k], v)
                    self.known[e][k] = v


class Builder:
    def __init__(self, cfg):
        self.c = cfg
        self.nc = bass.Bass("TRN2", target_bir_lowering=False)
        self.ins = {}

    def inp(self, name, shape):
        self.ins[name] = self.nc.dram_tensor(name, list(shape), F32, kind="ExternalInput").ap()
        return self.ins[name]

    def sb(self, es, name, shape, dt=F32):
        self._uid = getattr(self, "_uid", 0) + 1
        return Buf(es.enter_context(self.nc.sbuf_tensor("sb%d_%s" % (self._uid, name), list(shape), dt)))

    def build(self):
        c, nc = self.c, self.nc
        D, S, KC = c.D, c.S, c.KC
        L2 = c.DEPTH * 2
        x_in = self.inp("x", [S, D])
        self.inp("cT", [128, KC])
        self.inp("norm_g_bc", [L2, 128, D])
        self.inp("ada_w", [L2, D, 3 * D])
        self.inp("ada_b", [L2, 3 * D])
        self.inp("ident", [128, 128])
        self.inp("ones", [128, 128])
        self.inp("pool_w", [max(c.NP, 1), 4, c.PG, c.PG])
        self.inp("pool_scale_bc", [max(c.NP, 1), 128, D])
        self.inp("bands", [4, 3, 128, 128])
        self.inp("attn_w_in", [c.NA, D, c.QKV])
        self.inp("attn_w_o", [c.NA, D, D])
        self.inp("attn_gq_bc", [c.NA, 128, 64])
        self.inp("attn_gk_bc", [c.NA, 128, 64])
        self.inp("attn_sinks_bc", [c.NA, 128, c.H])
        self.inp("biasT", [128, c.H, 256])
        self.inp("maskT", [128, 256])
        NRm = max(c.NR, 1)
        self.inp("rw_w_rkv", [NRm, 3, D, D])
        self.inp("rw_w1", [NRm, D, c.LW])
        self.inp("rw_a1", [NRm, D, c.LA])
        self.inp("rw_g1", [NRm, D, c.LG])
        self.inp("rw_w2", [NRm, c.LW, D])
        self.inp("rw_a2", [NRm, c.LA, D])
        self.inp("rw_g2", [NRm, c.LG, D])
        self.inp("rw_w_o", [NRm, D, D])
        self.inp("rw_muT", [NRm, 128, 6, KC])
        for nm in ("w0", "a0", "k_k", "k_a", "r_k", "lnx_g", "lnx_b"):
            self.inp("rw_%s_bc" % nm, [NRm, 128, D])
        self.inp("fsel", [128, 64 * 128])
        self.inp("moe_w_r", [c.DEPTH, D, 36])
        self.inp("moe_b_r_bc", [c.DEPTH, 128, 36])
        self.inp("moe_w_gate", [c.DEPTH, c.NE, D, c.DE])
        self.inp("moe_w_up", [c.DEPTH, c.NE, D, c.DE])
        self.inp("moe_w_down", [c.DEPTH, c.NE, c.DE, D])
        y = nc.dram_tensor("y", [S, D], F32, kind="ExternalOutput").ap()
        xa = nc.dram_tensor("xa", [S, D], F32, kind="Internal").ap()
        xb = nc.dram_tensor("xb", [S, D], F32, kind="Internal").ap()
        self.scr = {nm: nc.dram_tensor("scr_" + nm, [S, D], F32, kind="Internal").ap()
                    for nm in ("r", "w", "k", "v", "a", "b", "g", "y")}
        self.scr["bon"] = nc.dram_tensor("scr_bon", [S, c.H], F32, kind="Internal").ap()

        with contextlib.ExitStack() as es:
            self.s = Sched(nc, es)
            s = self.s
            self.PSW = [Buf(es.enter_context(nc.psum_tensor("psw%d" % i, [128, 1024], F32))) for i in range(4)]
            self.PS = [Buf(self.PSW[i // 2].t[:, (i % 2) * 512:(i % 2 + 1) * 512]) for i in range(8)]
            self.ident = self.sb(es, "ident", [128, 128])
            self.ones = self.sb(es, "ones", [128, 128])
            self.A = self.sb(es, "modA", [128, D])
            self.Bsh = self.sb(es, "modB", [128, D])
            self.G = self.sb(es, "modG", [128, D])
            self.sc = self.sb(es, "sc", [128, KC])
            s.dma("sp", self.ident.t[:], self.ins["ident"], writes=[self.ident])
            s.dma("sp", self.ones.t[:], self.ins["ones"], writes=[self.ones])
            s.dma("sp", self.sc.t[:], self.ins["cT"], writes=[self.sc])
            s.op("act", lambda e: e.activation(out=self.sc.t[:], in_=self.sc.t[:], func=AF.Silu),
                 reads=[self.sc], writes=[self.sc])
            cur = x_in
            nsub_layers = c.DEPTH * 2
            k = 0
            for layer in range(c.DEPTH):
                kind, idx = layer % 3, layer // 3
                for sub in range(2):
                    k += 1
                    dst = y if k == nsub_layers else (xa if k % 2 == 1 else xb)
                    self.phase_mod(layer * 2 + sub)
                    s.barrier()
                    if sub == 1:
                        self.phase_moe(layer, cur, dst)
                    elif kind == 2:
                        self.phase_pool(idx, cur, dst)
                    elif kind == 0:
                        self.phase_attn(idx, cur, dst)
                    elif kind == 1:
                        self.phase_rw_proj(idx, cur)
                        s.barrier()
                        self.phase_rw_scan()
                        s.barrier()
                        self.phase_rw_out(idx, cur, dst)
                    else:
                        self.phase_copy(cur, dst)
                    s.barrier()
                    cur = dst
            s.barrier()
        return nc

    def phase_mod(self, ls):
        c, nc, s = self.c, self.nc, self.s
        D, KC = c.D, c.KC
        N3 = 3 * D
        with contextlib.ExitStack() as es:
            wst = [self.sb(es, "mod_w%d" % i, [128, KC, 512]) for i in range(2)]
            row = self.sb(es, "mod_row", [1, N3])
            brow = self.sb(es, "mod_brow", [1, N3])
            gbc = self.sb(es, "mod_gbc", [128, D])
            s.dma("sp", brow.t[:], self.ins["ada_b"][ls:ls + 1, :], writes=[brow])
            s.dma("sp", gbc.t[:], self.ins["norm_g_bc"][ls], writes=[gbc])
            wv = self.ins["ada_w"][ls].rearrange("(kc p) n -> p kc n", p=128)
            nch = N3 // 512
            for n in range(nch):
                w = wst[n % 2]
                s.dma("sp", w.t[:], wv[:, :, n * 512:(n + 1) * 512], writes=[w])
                ps = self.PS[n % 2]
                for kc in range(KC):
                    s.op("pe", lambda e, kc=kc, w=w, ps=ps: e.matmul(ps.t[0:1, :], lhsT=self.sc.t[:, kc:kc + 1],
                                                                      rhs=w.t[:, kc, :], start=(kc == 0), stop=(kc == KC - 1)),
                         reads=[self.sc, w], writes=[ps])
                s.op("dve", lambda e, n=n, ps=ps: e.tensor_tensor(out=row.t[0:1, n * 512:(n + 1) * 512], in0=ps.t[0:1, :],
                                                                   in1=brow.t[0:1, n * 512:(n + 1) * 512], op=ALU.add),
                     reads=[ps, brow], writes=[row])
            for j in range(nch):
                ps = self.PS[2 + j % 2]
                s.op("pe", lambda e, j=j, ps=ps: e.matmul(ps.t[:, :], lhsT=self.ones.t[0:1, :], rhs=row.t[0:1, j * 512:(j + 1) * 512],
                                                           start=True, stop=True), reads=[self.ones, row], writes=[ps])
                sec, off = divmod(j * 512, D)
                if sec == 0:
                    s.op("act", lambda e, ps=ps, off=off: e.copy(out=self.Bsh.t[:, off:off + 512], in_=ps.t[:, :]),
                         reads=[ps], writes=[self.Bsh])
                elif sec == 1:
                    s.op("dve", lambda e, ps=ps, off=off: e.scalar_tensor_tensor(
                        out=self.A.t[:, off:off + 512], in0=ps.t[:, :], scalar=1.0, in1=gbc.t[:, off:off + 512],
                        op0=ALU.add, op1=ALU.mult), reads=[ps, gbc], writes=[self.A])
                else:
                    s.op("act", lambda e, ps=ps, off=off: e.copy(out=self.G.t[:, off:off + 512], in_=ps.t[:, :]),
                         reads=[ps], writes=[self.G])
            s.barrier()

    def norm_tile(self, xt, h, ss):
        c, s = self.c, self.s
        s.op("act", lambda e: e.activation(out=h.t[:], in_=xt.t[:], func=AF.Square, accum_out=ss.t[:, 0:1]),
             reads=[xt], writes=[h, ss])
        s.op("dve", lambda e: e.tensor_scalar(out=ss.t[:, 1:2], in0=ss.t[:, 0:1], scalar1=1.0 / c.D, scalar2=1e-6,
                                              op0=ALU.mult, op1=ALU.add), reads=[ss], writes=[ss])
        s.op("act", lambda e: e.activation(out=ss.t[:, 1:2], in_=ss.t[:, 1:2], func=AF.Sqrt), reads=[ss], writes=[ss])
        s.op("dve", lambda e: e.reciprocal(out=ss.t[:, 1:2], in_=ss.t[:, 1:2]), reads=[ss], writes=[ss])
        s.op("dve", lambda e: e.scalar_tensor_tensor(out=h.t[:], in0=xt.t[:], scalar=ss.t[:, 1:2], in1=self.A.t[:],
                                                     op0=ALU.mult, op1=ALU.mult), reads=[xt, ss, self.A], writes=[h])
        s.op("dve", lambda e: e.tensor_tensor(out=h.t[:], in0=h.t[:], in1=self.Bsh.t[:], op=ALU.add),
             reads=[h, self.Bsh], writes=[h])

    def transpose_to(self, src, ncols, dsts, tok0, psums):
        s = self.s
        nk = ncols // 128
        g = 0
        for k0 in range(0, nk, 4):
            kn = min(4, nk - k0)
            ps = psums[g % len(psums)]
            g += 1
            for j in range(kn):
                s.op("pe", lambda e, j=j, k0=k0, ps=ps: e.transpose(ps.t[:, j * 128:(j + 1) * 128],
                                                                    src.t[:, (k0 + j) * 128:(k0 + j + 1) * 128], self.ident.t[:]),
                     reads=[src, self.ident], writes=[ps])
            for di, dst in enumerate(dsts):
                eng = "act" if di == 0 else "dve"
                if eng == "act":
                    s.op("act", lambda e, dst=dst, ps=ps, k0=k0, kn=kn: e.copy(
                        out=dst.t[:, k0:k0 + kn, tok0:tok0 + 128],
                        in_=ps.t[:, 0:kn * 128].rearrange("p (k t) -> p k t", t=128)), reads=[ps], writes=[dst])
                else:
                    s.op("dve", lambda e, dst=dst, ps=ps, k0=k0, kn=kn: e.tensor_copy(
                        out=dst.t[:, k0:k0 + kn, tok0:tok0 + 128],
                        in_=ps.t[:, 0:kn * 128].rearrange("p (k t) -> p k t", t=128)), reads=[ps], writes=[dst])

    def load_w(self, wap, K, N, wsts, wbf, scale_bc=None):
        s = self.s
        kcs = (K + 127) // 128
        rows = min(128, K)
        cap = wsts[0].t.shape[1]
        kper = max(1, min(kcs, cap // N))
        parts = [(k0, min(kcs, k0 + kper)) for k0 in range(0, kcs, kper)]
        for (k0, k1) in parts:
            self._wsti = getattr(self, "_wsti", 0) + 1
            wst = wsts[self._wsti % 2]
            n = (k1 - k0) * N
            if K >= 128:
                src = wap.rearrange("(kc p) n -> p kc n", p=128)[:, k0:k1, :]
                s.dma("sp", wst.t[:, 0:n].rearrange("p (kc n) -> p kc n", n=N), src, writes=[wst])
            else:
                s.dma("sp", wst.t[0:rows, 0:N], wap, writes=[wst])
            if scale_bc is None:
                s.op("pool", lambda e, wst=wst, n=n, k0=k0: e.tensor_copy(out=wbf.t[0:rows, k0 * N:k0 * N + n], in_=wst.t[0:rows, 0:n]),
                     reads=[wst], writes=[wbf])
            else:
                for kc in range(k0, k1):
                    s.op("pool", lambda e, kc=kc, wst=wst, k0=k0: e.tensor_tensor(
                        out=wbf.t[0:rows, kc * N:(kc + 1) * N], in0=wst.t[0:rows, (kc - k0) * N:(kc - k0 + 1) * N],
                        in1=scale_bc.t[0:rows, 0:N], op=ALU.mult), reads=[wst, scale_bc], writes=[wbf])

    def phase_copy(self, src, dst):
        c, s = self.c, self.s
        with contextlib.ExitStack() as es:
            t = [self.sb(es, "cp%d" % i, [128, c.D]) for i in range(2)]
            for i in range(c.S // 128):
                b = t[i % 2]
                s.dma("sp", b.t[:], src[i * 128:(i + 1) * 128, :], writes=[b])
                s.dma("act", dst[i * 128:(i + 1) * 128, :], b.t[:], reads=[b])

    def phase_pool(self, idx, src, dst):
        c, s = self.c, self.s
        D, KC, PG = c.D, c.KC, c.PG
        pk = PG // 128
        with contextlib.ExitStack() as es:
            xt = [self.sb(es, "pl_x%d" % i, [128, D]) for i in range(2)]
            h = self.sb(es, "pl_h", [128, D])
            hb = [self.sb(es, "pl_hb%d" % i, [128, D], BF16) for i in range(2)]
            ss = self.sb(es, "pl_ss", [128, 2])
            bst = self.sb(es, "pl_bst", [128, 12, 128])
            bands = self.sb(es, "pl_bands", [128, 12, 128], BF16)
            wst = self.sb(es, "pl_wst", [128, 4 * pk * PG])
            wbf = self.sb(es, "pl_wbf", [128, 4 * pk * PG], BF16)
            psc = self.sb(es, "pl_psc", [128, D])
            pT = [self.sb(es, "pl_pT%d" % i, [128, KC, 128], BF16) for i in range(2)]
            s.dma("sp", bst.t[:], self.ins["bands"].rearrange("w k p t -> p (w k) t"), writes=[bst])
            s.op("pool", lambda e: e.tensor_copy(out=bands.t[:], in_=bst.t[:]), reads=[bst], writes=[bands])
            s.dma("sp", psc.t[:], self.ins["pool_scale_bc"][idx], writes=[psc])
            s.dma("sp", wst.t[:, :].rearrange("p (g kc n) -> p g kc n", g=4, kc=pk),
                  self.ins["pool_w"][idx].rearrange("g (kc p) n -> p g kc n", p=128), writes=[wst])
            s.op("pool", lambda e: e.tensor_copy(out=wbf.t[:], in_=wst.t[:]), reads=[wst], writes=[wbf])
            s.op("dve", lambda e: e.tensor_tensor(out=psc.t[:], in0=psc.t[:], in1=self.G.t[:], op=ALU.mult),
                 reads=[psc, self.G], writes=[psc])
            for i in range(c.S // 128):
                x_ = xt[i % 2]
                hcur, hprev = hb[i % 2], hb[(i + 1) % 2]
                s.dma("sp", x_.t[:], src[i * 128:(i + 1) * 128, :], writes=[x_])
                self.norm_tile(x_, h, ss)
                s.op("act", lambda e, hcur=hcur: e.copy(out=hcur.t[:], in_=h.t[:]), reads=[h], writes=[hcur])
                p_ = pT[i % 2]
                for kc in range(KC):
                    g = (kc * 128) // PG
                    ps = self.PS[kc % 4]
                    bcur = g * 3 + (2 if i == 0 else 0)
                    s.op("pe", lambda e, kc=kc, ps=ps, bcur=bcur: e.matmul(
                        ps.t[:, 0:128], lhsT=hcur.t[:, kc * 128:(kc + 1) * 128], rhs=bands.t[:, bcur, :],
                        start=True, stop=(i == 0)), reads=[hcur, bands], writes=[ps])
                    if i > 0:
                        s.op("pe", lambda e, kc=kc, ps=ps, g=g: e.matmul(
                            ps.t[:, 0:128], lhsT=hprev.t[:, kc * 128:(kc + 1) * 128], rhs=bands.t[:, g * 3 + 1, :],
                            start=False, stop=True), reads=[hprev, bands], writes=[ps])
                    s.op("act", lambda e, kc=kc, ps=ps: e.copy(out=p_.t[:, kc, :], in_=ps.t[:, 0:128]),
                         reads=[ps], writes=[p_])
                for g in range(4):
                    ps = self.PS[4 + g % 4]
                    for kc in range(pk):
                        s.op("pe", lambda e, g=g, kc=kc, ps=ps: e.matmul(
                            ps.t[:, 0:PG], lhsT=p_.t[:, g * pk + kc, :],
                            rhs=wbf.t[:, (g * pk + kc) * PG:(g * pk + kc + 1) * PG],
                            start=(kc == 0), stop=(kc == pk - 1)), reads=[p_, wbf], writes=[ps])
                    s.op("dve", lambda e, g=g, ps=ps: e.tensor_tensor(out=h.t[:, g * PG:(g + 1) * PG], in0=ps.t[:, 0:PG],
                                                                      in1=psc.t[:, g * PG:(g + 1) * PG], op=ALU.mult),
                         reads=[ps, psc], writes=[h])
                s.op("dve", lambda e: e.tensor_tensor(out=x_.t[:], in0=x_.t[:], in1=h.t[:], op=ALU.add),
                     reads=[x_, h], writes=[x_])
                s.dma("act", dst[i * 128:(i + 1) * 128, :], x_.t[:], reads=[x_])

    def phase_attn(self, idx, src, dst):
        c, s = self.c, self.s
        D, KC, H, KV, GQ, QKV = c.D, c.KC, c.H, c.KV, c.GQ, c.QKV
        NTA = min(256, c.S)
        NS = NTA // 128
        QK = D + KV * 64
        with contextlib.ExitStack() as es:
            xs = [self.sb(es, "at_x%d" % i, [128, D]) for i in range(NS)]
            h = self.sb(es, "at_h", [128, D])
            ss = self.sb(es, "at_ss", [128, 2])
            hT = self.sb(es, "at_hT", [128, KC, NTA], BF16)
            oT = self.sb(es, "at_oT", [128, KC, NTA], BF16)
            qkv = [self.sb(es, "at_qkv%d" % i, [128, QKV]) for i in range(NS)]
            wst = [self.sb(es, "at_wst%d" % i, [128, KC * 256]) for i in range(2)]
            wbf = [self.sb(es, "at_wbf%d" % i, [128, KC * 512], BF16) for i in range(2)]
            biasT = self.sb(es, "at_bias", [128, H, 256])
            mask = self.sb(es, "at_mask", [128, 256])
            qn = self.sb(es, "at_qn", [128, QK])
            ssq = self.sb(es, "at_ssq", [128, H + KV])
            qT = self.sb(es, "at_qT", [64, H, 128], BF16)
            kT = [self.sb(es, "at_kT%d" % i, [64, KV, 128], BF16) for i in range(2)]
            vx = [self.sb(es, "at_vx%d" % i, [128, KV, 65], BF16) for i in range(2)]
            ssb = [self.sb(es, "at_ssb%d" % i, [128, 256]) for i in range(1)] * 2
            pf = [self.sb(es, "at_pf%d" % i, [128, 256]) for i in range(1)] * 2
            pT = [self.sb(es, "at_pT%d" % i, [128, 256], BF16) for i in range(2)]
            esink = self.sb(es, "at_esink", [128, H])
            gqk = self.sb(es, "at_gqk", [128, 64])
            gk_ = self.sb(es, "at_gk", [128, 64])
            den = self.sb(es, "at_den", [128, 2])
            s.dma("sp", biasT.t[:], self.ins["biasT"], writes=[biasT])
            s.dma("sp", mask.t[:], self.ins["maskT"], writes=[mask])
            s.dma("sp", esink.t[:], self.ins["attn_sinks_bc"][idx], writes=[esink])
            s.dma("sp", gqk.t[:], self.ins["attn_gq_bc"][idx], writes=[gqk])
            s.dma("sp", gk_.t[:], self.ins["attn_gk_bc"][idx], writes=[gk_])
            s.op("act", lambda e: e.activation(out=esink.t[:], in_=esink.t[:], func=AF.Exp), reads=[esink], writes=[esink])
            s.op("dve", lambda e: e.scalar_tensor_tensor(out=gqk.t[:], in0=gqk.t[:], scalar=0.125, in1=gk_.t[:],
                                                         op0=ALU.mult, op1=ALU.mult), reads=[gqk, gk_], writes=[gqk])
            for i in range(2):
                s.op("pool", lambda e, i=i: e.memset(vx[i].t[:], 1.0), writes=[vx[i]])
            w_in, w_o = self.ins["attn_w_in"][idx], self.ins["attn_w_o"][idx]
            blk = 0
            wi = 0
            for st in range(c.S // NTA):
                for sub in range(NS):
                    t0 = st * NTA + sub * 128
                    s.dma("sp", xs[sub].t[:], src[t0:t0 + 128, :], writes=[xs[sub]])
                    self.norm_tile(xs[sub], h, ss)
                    self.transpose_to(h, D, [hT], sub * 128, self.PS[4:8])
                q = 0
                for n0 in range(0, QKV, 512):
                    nw = min(512, QKV - n0)
                    wb = wbf[wi % 2]
                    wi += 1
                    self.load_w(w_in[:, n0:n0 + nw], D, nw, wst, wb)
                    for sub in range(NS):
                        ps = self.PS[q % 4]
                        q += 1
                        for kc in range(KC):
                            s.op("pe", lambda e, kc=kc, sub=sub, ps=ps, wb=wb, nw=nw: e.matmul(
                                ps.t[:, 0:nw], lhsT=hT.t[:, kc, sub * 128:(sub + 1) * 128], rhs=wb.t[:, kc * nw:(kc + 1) * nw],
                                start=(kc == 0), stop=(kc == KC - 1)), reads=[hT, wb], writes=[ps])
                        s.op("act", lambda e, sub=sub, ps=ps, n0=n0, nw=nw: e.copy(out=qkv[sub].t[:, n0:n0 + nw], in_=ps.t[:, 0:nw]),
                             reads=[ps], writes=[qkv[sub]])
                for sub in range(NS):
                    first = (blk == 0)
                    kc_, kp_ = kT[blk % 2], kT[(blk + 1) % 2]
                    vc_, vp_ = vx[blk % 2], vx[(blk + 1) % 2]
                    blk += 1
                    qk = qkv[sub]
                    s.op("dve", lambda e: e.tensor_tensor(out=qn.t[:, 0:QK], in0=qk.t[:, 0:QK], in1=qk.t[:, 0:QK], op=ALU.mult),
                         reads=[qk], writes=[qn])
                    s.op("dve", lambda e: e.tensor_reduce(out=ssq.t[:, :], in_=qn.t[:, 0:QK].rearrange("p (h d) -> p h d", d=64),
                                                          axis=AX.X, op=ALU.add), reads=[qn], writes=[ssq])
                    s.op("dve", lambda e: e.tensor_scalar(out=ssq.t[:, :], in0=ssq.t[:, :], scalar1=1.0 / 64, scalar2=1e-6,
                                                          op0=ALU.mult, op1=ALU.add), reads=[ssq], writes=[ssq])
                    s.op("act", lambda e: e.activation(out=ssq.t[:, :], in_=ssq.t[:, :], func=AF.Sqrt), reads=[ssq], writes=[ssq])
                    s.op("dve", lambda e: e.reciprocal(out=ssq.t[:, :], in_=ssq.t[:, :]), reads=[ssq], writes=[ssq])
                    for hh in range(H):
                        s.op("dve", lambda e, hh=hh: e.tensor_scalar(out=qn.t[:, hh * 64:(hh + 1) * 64], in0=qk.t[:, hh * 64:(hh + 1) * 64],
                                                                      scalar1=ssq.t[:, hh:hh + 1], scalar2=None, op0=ALU.mult),
                             reads=[qk, ssq], writes=[qn])
                    for kv in range(KV):
                        s.op("dve", lambda e, kv=kv: e.scalar_tensor_tensor(
                            out=qn.t[:, D + kv * 64:D + (kv + 1) * 64], in0=qk.t[:, D + kv * 64:D + (kv + 1) * 64],
                            scalar=ssq.t[:, H + kv:H + kv + 1], in1=gqk.t[:, :], op0=ALU.mult, op1=ALU.mult),
                            reads=[qk, ssq, gqk], writes=[qn])
                        s.op("act", lambda e, kv=kv: e.copy(out=vc_.t[:, kv, 0:64], in_=qk.t[:, QK + kv * 64:QK + (kv + 1) * 64]),
                             reads=[qk], writes=[vc_])
                    g = 0
                    for h0 in range(0, H + KV, 4):
                        hn = min(4, H + KV - h0)
                        ps = self.PS[g % 4]
                        g += 1
                        for j in range(hn):
                            s.op("pe", lambda e, j=j, h0=h0, ps=ps: e.transpose(ps.t[0:64, j * 128:(j + 1) * 128],
                                                                                qn.t[:, (h0 + j) * 64:(h0 + j + 1) * 64], self.ident.t[:]),
                                 reads=[qn, self.ident], writes=[ps])
                        for j in range(hn):
                            hh = h0 + j
                            dstv = qT.t[:, hh, :] if hh < H else kc_.t[:, hh - H, :]
                            dbuf = qT if hh < H else kc_
                            eng = "act" if j % 2 == 0 else "dve"
                            if eng == "act":
                                s.op("act", lambda e, j=j, ps=ps, dstv=dstv: e.copy(out=dstv, in_=ps.t[0:64, j * 128:(j + 1) * 128]),
                                     reads=[ps], writes=[dbuf])
                            else:
                                s.op("dve", lambda e, j=j, ps=ps, dstv=dstv: e.tensor_copy(out=dstv, in_=ps.t[0:64, j * 128:(j + 1) * 128]),
                                     reads=[ps], writes=[dbuf])
                    c0 = 128 if first else 0
                    for hh in range(H):
                        kv = hh // GQ
                        ps = self.PS[hh % 2]
                        po = self.PS[2 + (hh // 4) % 2]
                        j = hh % 4
                        if not first:
                            s.op("pe", lambda e, hh=hh, kv=kv, ps=ps: e.matmul(ps.t[:, 0:128], lhsT=kp_.t[:, kv, :], rhs=qT.t[:, hh, :],
                                                                               start=True, stop=True), reads=[kp_, qT], writes=[ps])
                        s.op("pe", lambda e, hh=hh, kv=kv, ps=ps: e.matmul(ps.t[:, 128:256], lhsT=kc_.t[:, kv, :], rhs=qT.t[:, hh, :],
                                                                           start=True, stop=True), reads=[kc_, qT], writes=[ps])
                        sb_, pf_, pT_ = ssb[hh % 2], pf[hh % 2], pT[hh % 2]
                        s.op("dve", lambda e, hh=hh, ps=ps, sb_=sb_: e.tensor_tensor(out=sb_.t[:, c0:256], in0=ps.t[:, c0:256],
                                                                                     in1=biasT.t[:, hh, c0:256], op=ALU.add),
                             reads=[ps, biasT], writes=[sb_])
                        s.op("act", lambda e, sb_=sb_, pf_=pf_: e.activation(out=pf_.t[:, c0:256], in_=sb_.t[:, c0:256], func=AF.Exp),
                             reads=[sb_], writes=[pf_])
                        s.op("pool", lambda e, pf_=pf_, pT_=pT_: e.tensor_tensor(out=pT_.t[:, c0:256], in0=pf_.t[:, c0:256],
                                                                                 in1=mask.t[:, c0:256], op=ALU.mult),
                             reads=[pf_, mask], writes=[pT_])
                        if not first:
                            s.op("pe", lambda e, kv=kv, po=po, j=j, pT_=pT_: e.matmul(po.t[:, j * 128:j * 128 + 65], lhsT=pT_.t[:, 0:128],
                                                                                      rhs=vp_.t[:, kv, :], start=True, stop=False),
                                 reads=[pT_, vp_], writes=[po])
                        s.op("pe", lambda e, kv=kv, po=po, j=j, pT_=pT_: e.matmul(po.t[:, j * 128:j * 128 + 65], lhsT=pT_.t[:, 128:256],
                                                                                  rhs=vc_.t[:, kv, :], start=first, stop=True),
                             reads=[pT_, vc_], writes=[po])
                        s.op("dve", lambda e, hh=hh, po=po, j=j: e.tensor_tensor(out=den.t[:, 0:1], in0=po.t[:, j * 128 + 64:j * 128 + 65],
                                                                                 in1=esink.t[:, hh:hh + 1], op=ALU.add),
                             reads=[po, esink], writes=[den])
                        s.op("dve", lambda e: e.reciprocal(out=den.t[:, 1:2], in_=den.t[:, 0:1]), reads=[den], writes=[den])
                        s.op("dve", lambda e, hh=hh, po=po, j=j: e.tensor_scalar(out=h.t[:, hh * 64:(hh + 1) * 64], in0=po.t[:, j * 128:j * 128 + 64],
                                                                                 scalar1=den.t[:, 1:2], scalar2=None, op0=ALU.mult),
                             reads=[po, den], writes=[h])
                    self.transpose_to(h, D, [oT], sub * 128, self.PS[4:8])
                q = 0
                for n0 in range(0, D, 512):
                    wb = wbf[wi % 2]
                    wi += 1
                    self.load_w(w_o[:, n0:n0 + 512], D, 512, wst, wb, scale_bc=None)
                    for sub in range(NS):
                        ps = self.PS[q % 4]
                        q += 1
                        for kc in range(KC):
                            s.op("pe", lambda e, kc=kc, sub=sub, ps=ps, wb=wb: e.matmul(
                                ps.t[:, :], lhsT=oT.t[:, kc, sub * 128:(sub + 1) * 128], rhs=wb.t[:, kc * 512:(kc + 1) * 512],
                                start=(kc == 0), stop=(kc == KC - 1)), reads=[oT, wb], writes=[ps])
                        s.op("dve", lambda e, sub=sub, ps=ps, n0=n0: e.tensor_tensor(out=h.t[:, 0:512], in0=ps.t[:, :],
                                                                                     in1=self.G.t[:, n0:n0 + 512], op=ALU.mult),
                             reads=[ps, self.G], writes=[h])
                        s.op("dve", lambda e, sub=sub, n0=n0: e.tensor_tensor(out=xs[sub].t[:, n0:n0 + 512], in0=xs[sub].t[:, n0:n0 + 512],
                                                                              in1=h.t[:, 0:512], op=ALU.add),
                             reads=[h, xs[sub]], writes=[xs[sub]])
                for sub in range(NS):
                    t0 = st * NTA + sub * 128
                    s.dma("act", dst[t0:t0 + 128, :], xs[sub].t[:], reads=[xs[sub]])

    def phase_rw_proj(self, idx, src):
        c, s = self.c, self.s
        D, KC, H, LW, LA, LG = c.D, c.KC, c.H, c.LW, c.LA, c.LG
        LGC = (LG + 127) // 128
        lgr = min(128, LG)
        I = self.ins
        perm = lambda ap: ap.rearrange("t (hh j k) -> t j hh k", hh=2, k=64)
        with contextlib.ExitStack() as es:
            x_ = self.sb(es, "rp_x", [128, D])
            h = self.sb(es, "rp_h", [128, D])
            ss = self.sb(es, "rp_ss", [128, 2])
            hT = self.sb(es, "rp_hT", [128, KC, 129])
            xx = self.sb(es, "rp_xx", [128, KC, 128])
            xm = [self.sb(es, "rp_xm%d" % i, [128, KC, 128], BF16) for i in range(4)]
            wst = [self.sb(es, "rp_wst%d" % i, [128, max(KC * 128, D)]) for i in range(2)]
            wbf = [self.sb(es, "rp_wbf%d" % i, [128, KC * 512], BF16) for i in range(3)]
            w1b = self.sb(es, "rp_w1b", [128, KC * LW], BF16)
            a1b = self.sb(es, "rp_a1b", [128, KC * LA], BF16)
            g1b = self.sb(es, "rp_g1b", [128, KC * LG], BF16)
            w2b = self.sb(es, "rp_w2b", [128, D], BF16)
            a2b = self.sb(es, "rp_a2b", [128, D], BF16)
            g2b = self.sb(es, "rp_g2b", [128, LGC * D], BF16)
            muT = self.sb(es, "rp_muT", [128, 6, KC])
            twT = self.sb(es, "rp_twT", [128, 128], BF16)
            taT = self.sb(es, "rp_taT", [128, 128], BF16)
            tgT = self.sb(es, "rp_tgT", [128, LGC, 128], BF16)
            bcn = ("w0", "a0", "k_k", "k_a", "r_k")
            bcc = {nm: self.sb(es, "rp_bc_" + nm, [128, 512]) for nm in bcn}
            E = {nm: self.sb(es, "rp_e_" + nm, [128, 512]) for nm in
                 ("r", "v", "g", "zw", "a", "k", "kk", "kkn", "bs", "k2", "u")}
            E["sq"] = E["u"]
            E["t"] = E["u"]
            E["as"] = E["kk"]
            sm = self.sb(es, "rp_sm", [128, 16])
            bon = self.sb(es, "rp_bon", [128, H])
            wstbig = wst[0]
            for (nm, dstb, K_, N_) in (("rw_w1", w1b, D, LW), ("rw_a1", a1b, D, LA), ("rw_g1", g1b, D, LG),
                                       ("rw_w2", w2b, LW, D), ("rw_a2", a2b, LA, D)):
                self.load_w(I[nm][idx], K_, N_, wst, dstb)
            for lc in range(LGC):
                r_ = min(128, LG - lc * 128)
                s.dma("sp", wstbig.t[0:r_, 0:D], I["rw_g2"][idx][lc * 128:lc * 128 + r_, :], writes=[wstbig])
                s.op("pool", lambda e, lc=lc, r_=r_: e.tensor_copy(out=g2b.t[0:r_, lc * D:(lc + 1) * D], in_=wstbig.t[0:r_, 0:D]),
                     reads=[wstbig], writes=[g2b])
            s.dma("sp", muT.t[:], I["rw_muT"][idx], writes=[muT])
            s.op("dve", lambda e: e.memset(hT.t[:, :, 0:1], 0.0), writes=[hT])
            wr_ = I["rw_w_rkv"][idx]
            for i in range(c.S // 128):
                t0 = i * 128
                s.dma("sp", x_.t[:], src[t0:t0 + 128, :], writes=[x_])
                self.norm_tile(x_, h, ss)
                g = 0
                for k0 in range(0, KC, 4):
                    kn = min(4, KC - k0)
                    ps = self.PS[4 + g % 4]
                    g += 1
                    for j in range(kn):
                        s.op("pe", lambda e, j=j, k0=k0, ps=ps: e.transpose(ps.t[:, j * 128:(j + 1) * 128],
                                                                            h.t[:, (k0 + j) * 128:(k0 + j + 1) * 128], self.ident.t[:]),
                             reads=[h, self.ident], writes=[ps])
                    s.op("act", lambda e, ps=ps, k0=k0, kn=kn: e.copy(
                        out=hT.t[:, k0:k0 + kn, 1:129], in_=ps.t[:, 0:kn * 128].rearrange("p (k t) -> p k t", t=128)),
                        reads=[ps], writes=[hT])
                s.op("dve", lambda e: e.tensor_tensor(out=xx.t[:], in0=hT.t[:, :, 0:128], in1=hT.t[:, :, 1:129], op=ALU.subtract),
                     reads=[hT], writes=[xx])

                def mix(m, dstb):
                    for kc in range(KC):
                        s.op("dve", lambda e, kc=kc: e.scalar_tensor_tensor(
                            out=dstb.t[:, kc, :], in0=xx.t[:, kc, :], scalar=muT.t[:, m, kc:kc + 1], in1=hT.t[:, kc, 1:129],
                            op0=ALU.mult, op1=ALU.add), reads=[xx, muT, hT], writes=[dstb])

                mix(3, xm[3])
                ps = self.PS[0]
                for kc in range(KC):
                    s.op("pe", lambda e, kc=kc: e.matmul(ps.t[0:LW, 0:128], lhsT=w1b.t[:, kc * LW:(kc + 1) * LW], rhs=xm[3].t[:, kc, :],
                                                          start=(kc == 0), stop=(kc == KC - 1)), reads=[w1b, xm[3]], writes=[ps])
                s.op("act", lambda e: e.activation(out=twT.t[0:LW, :], in_=ps.t[0:LW, 0:128], func=AF.Tanh), reads=[ps], writes=[twT])
                mix(4, xm[3])
                ps = self.PS[1]
                for kc in range(KC):
                    s.op("pe", lambda e, kc=kc: e.matmul(ps.t[0:LA, 0:128], lhsT=a1b.t[:, kc * LA:(kc + 1) * LA], rhs=xm[3].t[:, kc, :],
                                                          start=(kc == 0), stop=(kc == KC - 1)), reads=[a1b, xm[3]], writes=[ps])
                s.op("act", lambda e: e.copy(out=taT.t[0:LA, :], in_=ps.t[0:LA, 0:128]), reads=[ps], writes=[taT])
                mix(5, xm[3])
                for lc in range(LGC):
                    r_ = min(128, LG - lc * 128)
                    ps = self.PS[2 + lc % 2]
                    for kc in range(KC):
                        s.op("pe", lambda e, kc=kc, lc=lc, r_=r_, ps=ps: e.matmul(
                            ps.t[0:r_, 0:128], lhsT=g1b.t[:, kc * LG + lc * 128: kc * LG + lc * 128 + r_], rhs=xm[3].t[:, kc, :],
                            start=(kc == 0), stop=(kc == KC - 1)), reads=[g1b, xm[3]], writes=[ps])
                    s.op("act", lambda e, lc=lc, r_=r_, ps=ps: e.activation(out=tgT.t[0:r_, lc, :], in_=ps.t[0:r_, 0:128], func=AF.Sigmoid),
                         reads=[ps], writes=[tgT])
                for m in range(3):
                    mix(m, xm[m])
                for n0 in range(0, D, 512):
                    sl = slice(n0, n0 + 512)
                    for m in range(3):
                        self.load_w(wr_[m][:, sl], D, 512, wst, wbf[m])
                    for nm in bcn:
                        s.dma("sp", bcc[nm].t[:], I["rw_%s_bc" % nm][idx][:, sl], writes=[bcc[nm]])
                    Pr, Pk, Pv, Pw, Pa, Pg = (self.PS[j] for j in range(6))
                    for m, P_ in ((0, Pr), (1, Pk), (2, Pv)):
                        for kc in range(KC):
                            s.op("pe", lambda e, kc=kc, m=m, P_=P_: e.matmul(P_.t[:, :], lhsT=xm[m].t[:, kc, :],
                                                                             rhs=wbf[m].t[:, kc * 512:(kc + 1) * 512],
                                                                             start=(kc == 0), stop=(kc == KC - 1)),
                                 reads=[xm[m], wbf[m]], writes=[P_])
                    s.op("pe", lambda e: e.matmul(Pw.t[:, :], lhsT=twT.t[0:LW, :], rhs=w2b.t[0:LW, sl], start=True, stop=True),
                         reads=[twT, w2b], writes=[Pw])
                    s.op("pe", lambda e: e.matmul(Pa.t[:, :], lhsT=taT.t[0:LA, :], rhs=a2b.t[0:LA, sl], start=True, stop=True),
                         reads=[taT, a2b], writes=[Pa])
                    for lc in range(LGC):
                        r_ = min(128, LG - lc * 128)
                        s.op("pe", lambda e, lc=lc, r_=r_: e.matmul(Pg.t[:, :], lhsT=tgT.t[0:r_, lc, :],
                                                                    rhs=g2b.t[0:r_, lc * D + n0: lc * D + n0 + 512],
                                                                    start=(lc == 0), stop=(lc == LGC - 1)), reads=[tgT, g2b], writes=[Pg])
                    A_ = lambda fn, rd, wr: s.op("act", fn, reads=rd, writes=wr)
                    V_ = lambda fn, rd, wr: s.op("dve", fn, reads=rd, writes=wr)
                    G_ = lambda fn, rd, wr: s.op("pool", fn, reads=rd, writes=wr)
                    j0 = n0 // 128
                    def pstore(nm, b):
                        for hh in range(2):
                            s.dma("act", perm(self.scr[nm][t0:t0 + 128, :])[:, j0:j0 + 4, hh, :],
                                  b.t[:, :].rearrange("p (j hh k) -> p j hh k", hh=2, k=64)[:, :, hh, :], reads=[b])
                    A_(lambda e: e.copy(out=E["r"].t[:], in_=Pr.t[:, :]), [Pr], [E["r"]])
                    pstore("r", E["r"])
                    A_(lambda e: e.copy(out=E["v"].t[:], in_=Pv.t[:, :]), [Pv], [E["v"]])
                    s.dma("act", self.scr["v"][t0:t0 + 128, sl], E["v"].t[:], reads=[E["v"]])
                    A_(lambda e: e.copy(out=E["g"].t[:], in_=Pg.t[:, :]), [Pg], [E["g"]])
                    s.dma("act", self.scr["g"][t0:t0 + 128, sl], E["g"].t[:], reads=[E["g"]])
                    V_(lambda e: e.tensor_tensor(out=E["zw"].t[:], in0=Pw.t[:, :], in1=bcc["w0"].t[:], op=ALU.add), [Pw, bcc["w0"]], [E["zw"]])
                    A_(lambda e: e.activation(out=E["zw"].t[:], in_=E["zw"].t[:], func=AF.Sigmoid), [E["zw"]], [E["zw"]])
                    A_(lambda e: e.activation(out=E["zw"].t[:], in_=E["zw"].t[:], func=AF.Exp, scale=-math.exp(-0.5)), [E["zw"]], [E["zw"]])
                    pstore("w", E["zw"])
                    V_(lambda e: e.tensor_tensor(out=E["a"].t[:], in0=Pa.t[:, :], in1=bcc["a0"].t[:], op=ALU.add), [Pa, bcc["a0"]], [E["a"]])
                    A_(lambda e: e.activation(out=E["a"].t[:], in_=E["a"].t[:], func=AF.Sigmoid), [E["a"]], [E["a"]])
                    A_(lambda e: e.copy(out=E["k"].t[:], in_=Pk.t[:, :]), [Pk], [E["k"]])
                    G_(lambda e: e.tensor_tensor(out=E["kk"].t[:], in0=E["k"].t[:], in1=bcc["k_k"].t[:], op=ALU.mult), [E["k"], bcc["k_k"]], [E["kk"]])
                    G_(lambda e: e.tensor_tensor(out=E["sq"].t[:], in0=E["kk"].t[:], in1=E["kk"].t[:], op=ALU.mult), [E["kk"]], [E["sq"]])
                    V_(lambda e: e.tensor_reduce(out=sm.t[:, 0:8], in_=E["sq"].t[:, :].rearrange("p (h d) -> p h d", d=64), axis=AX.X, op=ALU.add),
                       [E["sq"]], [sm])
                    A_(lambda e: e.activation(out=sm.t[:, 0:8], in_=sm.t[:, 0:8], func=AF.Sqrt), [sm], [sm])
                    V_(lambda e: e.tensor_scalar(out=sm.t[:, 0:8], in0=sm.t[:, 0:8], scalar1=1e-12, scalar2=None, op0=ALU.max), [sm], [sm])
                    V_(lambda e: e.reciprocal(out=sm.t[:, 0:8], in_=sm.t[:, 0:8]), [sm], [sm])
                    V_(lambda e: e.tensor_tensor(out=E["kkn"].t[:, :].rearrange("p (h d) -> p h d", d=64),
                                                 in0=E["kk"].t[:, :].rearrange("p (h d) -> p h d", d=64),
                                                 in1=sm.t[:, 0:8].unsqueeze(2).to_broadcast([128, 8, 64]), op=ALU.mult), [E["kk"], sm], [E["kkn"]])
                    A_(lambda e: e.mul(out=E["as"].t[:], in_=E["kkn"].t[:], mul=-1.0), [E["kkn"]], [E["as"]])
                    pstore("a", E["as"])
                    G_(lambda e: e.tensor_tensor(out=E["bs"].t[:], in0=E["kkn"].t[:], in1=E["a"].t[:], op=ALU.mult), [E["kkn"], E["a"]], [E["bs"]])
                    pstore("b", E["bs"])
                    V_(lambda e: e.scalar_tensor_tensor(out=E["t"].t[:], in0=E["a"].t[:], scalar=-1.0, in1=bcc["k_a"].t[:],
                                                        op0=ALU.add, op1=ALU.mult), [E["a"], bcc["k_a"]], [E["t"]])
                    V_(lambda e: e.scalar_tensor_tensor(out=E["k2"].t[:], in0=E["t"].t[:], scalar=1.0, in1=E["k"].t[:],
                                                        op0=ALU.add, op1=ALU.mult), [E["t"], E["k"]], [E["k2"]])
                    pstore("k", E["k2"])
                    G_(lambda e: e.tensor_tensor(out=E["u"].t[:], in0=E["r"].t[:], in1=E["k2"].t[:], op=ALU.mult), [E["r"], E["k2"]], [E["u"]])
                    G_(lambda e: e.tensor_tensor(out=E["u"].t[:], in0=E["u"].t[:], in1=bcc["r_k"].t[:], op=ALU.mult), [E["u"], bcc["r_k"]], [E["u"]])
                    hb = n0 // 64
                    V_(lambda e: e.tensor_reduce(out=bon.t[:, hb:hb + 8], in_=E["u"].t[:, :].rearrange("p (h d) -> p h d", d=64),
                                                 axis=AX.X, op=ALU.add), [E["u"]], [bon])
                s.dma("act", self.scr["bon"][t0:t0 + 128, :], bon.t[:], reads=[bon])
                s.op("dve", lambda e: e.tensor_copy(out=hT.t[:, :, 0:1], in_=hT.t[:, :, 128:129]), reads=[hT], writes=[hT])

    def phase_rw_scan(self):
        c, s = self.c, self.s
        D, H = c.D, c.H
        NPR = H // 2
        RW = D // 2
        NCH = (RW + 511) // 512
        CW = min(512, RW)
        with contextlib.ExitStack() as es:
            F = self.sb(es, "sc_F", [128, 64 * 128])
            rows = {nm: [self.sb(es, "sc_row_%s%d" % (nm, i), [128, RW]) for i in range(2)] for nm in ("a", "w", "b", "k", "r")}
            vt = self.sb(es, "sc_vt", [128, D])
            vcol = [self.sb(es, "sc_vcol%d" % i, [128, NPR, 128]) for i in range(2)]
            ybuf = [self.sb(es, "sc_ybuf%d" % i, [128, NPR, 128]) for i in range(2)]
            yt = [self.sb(es, "sc_yt%d" % i, [128, D]) for i in range(2)]
            St = self.sb(es, "sc_S", [128, RW])
            T = [self.sb(es, "sc_T%d" % i, [128, RW]) for i in range(2)]
            sa = self.sb(es, "sc_sa", [128, NPR])
            s.dma("sp", F.t[:], self.ins["fsel"], writes=[F])
            s.op("dve", lambda e: e.memset(St.t[:], 0.0), writes=[St])
            v3 = lambda ap: ap.rearrange("p (j k) -> p j k", k=64)
            slot = 0
            for bi in range(c.S // 128):
                t0 = bi * 128
                vc, yb = vcol[bi % 2], ybuf[bi % 2]
                s.dma("sp", vt.t[:], self.scr["v"][t0:t0 + 128, :], writes=[vt])
                for j in range(NPR):
                    ps = self.PS[j % 2]
                    s.op("pe", lambda e, j=j, ps=ps: e.transpose(ps.t[:, 0:128], vt.t[:, j * 128:(j + 1) * 128], self.ident.t[:]),
                         reads=[vt, self.ident], writes=[ps])
                    s.op("act", lambda e, j=j, ps=ps: e.copy(out=vc.t[:, j, :], in_=ps.t[:, 0:128]), reads=[ps], writes=[vc])
                for half in range(2):
                    hb = (bi * 2 + half) % 2
                    tb = t0 + half * 64
                    for nm in ("a", "w", "b", "k", "r"):
                        s.dma("sp", rows[nm][hb].t[:], self.scr[nm][tb:tb + 64, :].rearrange("t (hh x) -> (t hh) x", hh=2),
                              writes=[rows[nm][hb]])
                    for tt in range(64):
                        tl = half * 64 + tt
                        sel = F.t[:, tt * 128:(tt + 1) * 128]

                        def bcast(nm):
                            nonlocal slot
                            pw = self.PSW[slot % 4]
                            slot += 1
                            for ch in range(NCH):
                                s.op("pe", lambda e, ch=ch, pw=pw: e.matmul(pw.t[:, ch * 512:ch * 512 + CW], lhsT=sel,
                                                                            rhs=rows[nm][hb].t[:, ch * 512:ch * 512 + CW],
                                                                            start=True, stop=True),
                                     reads=[F, rows[nm][hb]], writes=[pw])
                            return pw
                        V_ = lambda fn, rd, wr: s.op("dve", fn, reads=rd, writes=wr)
                        pa = bcast("a")
                        pw_ = bcast("w")
                        pb = bcast("b")
                        pk = bcast("k")
                        T0, T1 = T[0], T[1]
                        V_(lambda e: e.tensor_tensor(out=T0.t[:], in0=St.t[:], in1=pa.t[:, 0:RW], op=ALU.mult), [St, pa], [T0])
                        V_(lambda e: e.tensor_reduce(out=sa.t[:, :], in_=v3(T0.t[:, :]), axis=AX.X, op=ALU.add), [T0], [sa])
                        V_(lambda e: e.tensor_tensor(out=St.t[:], in0=St.t[:], in1=pw_.t[:, 0:RW], op=ALU.mult), [St, pw_], [St])
                        V_(lambda e: e.tensor_tensor(out=v3(T1.t[:, :]), in0=v3(pb.t[:, 0:RW]),
                                                     in1=sa.t[:, :].unsqueeze(2).to_broadcast([128, NPR, 64]), op=ALU.mult), [pb, sa], [T1])
                        V_(lambda e: e.tensor_tensor(out=St.t[:], in0=St.t[:], in1=T1.t[:], op=ALU.add), [St, T1], [St])
                        V_(lambda e: e.tensor_tensor(out=v3(T0.t[:, :]), in0=v3(pk.t[:, 0:RW]),
                                                     in1=vc.t[:, :, tl].unsqueeze(2).to_broadcast([128, NPR, 64]), op=ALU.mult), [pk, vc], [T0])
                        pr = bcast("r")
                        V_(lambda e: e.tensor_tensor(out=St.t[:], in0=St.t[:], in1=T0.t[:], op=ALU.add), [St, T0], [St])
                        V_(lambda e: e.tensor_tensor(out=T1.t[:], in0=St.t[:], in1=pr.t[:, 0:RW], op=ALU.mult), [St, pr], [T1])
                        V_(lambda e: e.tensor_reduce(out=yb.t[:, :, tl], in_=v3(T1.t[:, :]), axis=AX.X, op=ALU.add), [T1], [yb])
                yo = yt[bi % 2]
                for j in range(NPR):
                    ps = self.PS[j % 2]
                    s.op("pe", lambda e, j=j, ps=ps: e.transpose(ps.t[:, 0:128], yb.t[:, j, :], self.ident.t[:]),
                         reads=[yb, self.ident], writes=[ps])
                    s.op("act", lambda e, j=j, ps=ps: e.copy(out=yo.t[:, j * 128:(j + 1) * 128], in_=ps.t[:, 0:128]), reads=[ps], writes=[yo])
                s.dma("act", self.scr["y"][t0:t0 + 128, :], yo.t[:], reads=[yo])

    def phase_rw_out(self, idx, src, dst):
        c, s = self.c, self.s
        D, KC, H = c.D, c.KC, c.H
        NT3 = min(256, c.S)
        NS = NT3 // 128
        I = self.ins
        h3 = lambda ap: ap.rearrange("p (h d) -> p h d", d=64)
        bc3 = lambda ap: ap.unsqueeze(2).to_broadcast([128, H, 64])
        with contextlib.ExitStack() as es:
            xs = [self.sb(es, "ro_x%d" % i, [128, D]) for i in range(NS)]
            y_ = self.sb(es, "ro_y", [128, D])
            v_ = self.sb(es, "ro_v", [128, D])
            g_ = self.sb(es, "ro_g", [128, D])
            tm = self.sb(es, "ro_tm", [128, D])
            lng = self.sb(es, "ro_lng", [128, D])
            lnb = self.sb(es, "ro_lnb", [128, D])
            bon = self.sb(es, "ro_bon", [128, H])
            st_ = self.sb(es, "ro_st", [128, 2 * H])
            zT = self.sb(es, "ro_zT", [128, KC, NT3], BF16)
            wst = [self.sb(es, "ro_wst%d" % i, [128, KC * 256]) for i in range(2)]
            wbf = [self.sb(es, "ro_wbf%d" % i, [128, KC * 512], BF16) for i in range(2)]
            s.dma("sp", lng.t[:], I["rw_lnx_g_bc"][idx], writes=[lng])
            s.dma("sp", lnb.t[:], I["rw_lnx_b_bc"][idx], writes=[lnb])
            V_ = lambda fn, rd, wr: s.op("dve", fn, reads=rd, writes=wr)
            G_ = lambda fn, rd, wr: s.op("pool", fn, reads=rd, writes=wr)
            wi = 0
            for st in range(c.S // NT3):
                for sub in range(NS):
                    t0 = st * NT3 + sub * 128
                    s.dma("sp", xs[sub].t[:], src[t0:t0 + 128, :], writes=[xs[sub]])
                    s.dma("sp", y_.t[:], self.scr["y"][t0:t0 + 128, :], writes=[y_])
                    s.dma("sp", v_.t[:], self.scr["v"][t0:t0 + 128, :], writes=[v_])
                    s.dma("sp", g_.t[:], self.scr["g"][t0:t0 + 128, :], writes=[g_])
                    s.dma("sp", bon.t[:], self.scr["bon"][t0:t0 + 128, :], writes=[bon])
                    V_(lambda e: e.tensor_reduce(out=st_.t[:, 0:H], in_=h3(y_.t[:, :]), axis=AX.X, op=ALU.add), [y_], [st_])
                    V_(lambda e: e.tensor_scalar(out=st_.t[:, 0:H], in0=st_.t[:, 0:H], scalar1=1.0 / 64, scalar2=None, op0=ALU.mult), [st_], [st_])
                    V_(lambda e: e.tensor_tensor(out=h3(y_.t[:, :]), in0=h3(y_.t[:, :]), in1=bc3(st_.t[:, 0:H]), op=ALU.subtract), [y_, st_], [y_])
                    G_(lambda e: e.tensor_tensor(out=tm.t[:], in0=y_.t[:], in1=y_.t[:], op=ALU.mult), [y_], [tm])
                    V_(lambda e: e.tensor_reduce(out=st_.t[:, H:2 * H], in_=h3(tm.t[:, :]), axis=AX.X, op=ALU.add), [tm], [st_])
                    V_(lambda e: e.tensor_scalar(out=st_.t[:, H:2 * H], in0=st_.t[:, H:2 * H], scalar1=1.0 / 64, scalar2=64e-5,
                                                 op0=ALU.mult, op1=ALU.add), [st_], [st_])
                    s.op("act", lambda e: e.activation(out=st_.t[:, H:2 * H], in_=st_.t[:, H:2 * H], func=AF.Sqrt), reads=[st_], writes=[st_])
                    V_(lambda e: e.reciprocal(out=st_.t[:, H:2 * H], in_=st_.t[:, H:2 * H]), [st_], [st_])
                    V_(lambda e: e.tensor_tensor(out=h3(y_.t[:, :]), in0=h3(y_.t[:, :]), in1=bc3(st_.t[:, H:2 * H]), op=ALU.mult), [y_, st_], [y_])
                    G_(lambda e: e.tensor_tensor(out=y_.t[:], in0=y_.t[:], in1=lng.t[:], op=ALU.mult), [y_, lng], [y_])
                    G_(lambda e: e.tensor_tensor(out=y_.t[:], in0=y_.t[:], in1=lnb.t[:], op=ALU.add), [y_, lnb], [y_])
                    V_(lambda e: e.tensor_tensor(out=h3(tm.t[:, :]), in0=h3(v_.t[:, :]), in1=bc3(bon.t[:, 0:H]), op=ALU.mult), [v_, bon], [tm])
                    G_(lambda e: e.tensor_tensor(out=y_.t[:], in0=y_.t[:], in1=tm.t[:], op=ALU.add), [y_, tm], [y_])
                    V_(lambda e: e.tensor_tensor(out=tm.t[:], in0=y_.t[:], in1=g_.t[:], op=ALU.mult), [y_, g_], [tm])
                    self.transpose_to(tm, D, [zT], sub * 128, self.PS[4:8])
                q = 0
                for n0 in range(0, D, 512):
                    wb = wbf[wi % 2]
                    wi += 1
                    self.load_w(I["rw_w_o"][idx][:, n0:n0 + 512], D, 512, wst, wb)
                    for sub in range(NS):
                        ps = self.PS[q % 4]
                        q += 1
                        for kc in range(KC):
                            s.op("pe", lambda e, kc=kc, sub=sub, ps=ps, wb=wb: e.matmul(
                                ps.t[:, :], lhsT=zT.t[:, kc, sub * 128:(sub + 1) * 128], rhs=wb.t[:, kc * 512:(kc + 1) * 512],
                                start=(kc == 0), stop=(kc == KC - 1)), reads=[zT, wb], writes=[ps])
                        V_(lambda e, ps=ps, n0=n0: e.tensor_tensor(out=tm.t[:, 0:512], in0=ps.t[:, :], in1=self.G.t[:, n0:n0 + 512], op=ALU.mult),
                           [ps, self.G], [tm])
                        V_(lambda e, sub=sub, n0=n0: e.tensor_tensor(out=xs[sub].t[:, n0:n0 + 512], in0=xs[sub].t[:, n0:n0 + 512],
                                                                     in1=tm.t[:, 0:512], op=ALU.add), [tm, xs[sub]], [xs[sub]])
                for sub in range(NS):
                    t0 = st * NT3 + sub * 128
                    s.dma("act", dst[t0:t0 + 128, :], xs[sub].t[:], reads=[xs[sub]])

    def phase_moe(self, layer, src, dst):
        c, s = self.c, self.s
        D, KC, DE, NT, NSUB = c.D, c.KC, c.DE, c.NT, c.NSUB
        fk = DE // 128
        WSZ = max(KC * DE, fk * D)
        with contextlib.ExitStack() as es:
            xs = [self.sb(es, "mo_x%d" % i, [128, D]) for i in range(NSUB)]
            h = self.sb(es, "mo_h", [128, D])
            ss = self.sb(es, "mo_ss", [128, 2])
            hT = self.sb(es, "mo_hT", [128, KC, NT], BF16)
            hT32 = self.sb(es, "mo_hT32", [128, KC, 128])
            wr = self.sb(es, "mo_wr", [128, KC, 36])
            brb = self.sb(es, "mo_brb", [128, 36])
            wst = [self.sb(es, "mo_wst%d" % i, [128, max(WSZ // 2, D if fk == 1 else 0)]) for i in range(2)]
            wbf = [self.sb(es, "mo_wbf%d" % i, [128, WSZ], BF16) for i in range(3)]
            hid = [self.sb(es, "mo_hid%d" % i, [128, fk, NT], BF16) for i in range(2)]
            sg = [self.sb(es, "mo_sg%d" % i, [128, NT]) for i in range(2)]
            coef = [self.sb(es, "mo_coef%d" % i, [128, 32]) for i in range(NSUB)]
            rt = self.sb(es, "mo_rt", [128, 160])
            s.dma("sp", wr.t[:], self.ins["moe_w_r"][layer].rearrange("(kc p) n -> p kc n", p=128), writes=[wr])
            s.dma("sp", brb.t[:], self.ins["moe_b_r_bc"][layer], writes=[brb])
            wi = 0
            for st in range(c.S // NT):
                for sub in range(NSUB):
                    t0 = st * NT + sub * 128
                    x_ = xs[sub]
                    s.dma("sp", x_.t[:], src[t0:t0 + 128, :], writes=[x_])
                    self.norm_tile(x_, h, ss)
                    self.transpose_to(h, D, [hT], sub * 128, self.PS[4:8])
                    self._transpose32(h, hT32)
                    self.router(hT32, wr, brb, rt, coef[sub])
                for e_ in range(c.NE):
                    wg, wu, wd = wbf[wi % 3], wbf[(wi + 1) % 3], wbf[(wi + 2) % 3]
                    self.load_w(self.ins["moe_w_gate"][layer, e_], D, DE, wst, wg)
                    self.load_w(self.ins["moe_w_up"][layer, e_], D, DE, wst, wu)
                    self.load_w(self.ins["moe_w_down"][layer, e_], DE, D, wst, wd, scale_bc=self.G)
                    wi += 3
                    hd = hid[e_ % 2]
                    for fc in range(fk):
                        pg, pu = self.PS[(fc % 2) * 2], self.PS[(fc % 2) * 2 + 1]
                        for kc in range(KC):
                            s.op("pe", lambda e, kc=kc, fc=fc, pg=pg, wg=wg: e.matmul(
                                pg.t[:, 0:NT], lhsT=wg.t[:, kc * DE + fc * 128: kc * DE + (fc + 1) * 128], rhs=hT.t[:, kc, :],
                                start=(kc == 0), stop=(kc == KC - 1)), reads=[wg, hT], writes=[pg])
                        for kc in range(KC):
                            s.op("pe", lambda e, kc=kc, fc=fc, pu=pu, wu=wu: e.matmul(
                                pu.t[:, 0:NT], lhsT=wu.t[:, kc * DE + fc * 128: kc * DE + (fc + 1) * 128], rhs=hT.t[:, kc, :],
                                start=(kc == 0), stop=(kc == KC - 1)), reads=[wu, hT], writes=[pu])
                        sg_ = sg[fc % 2]
                        s.op("act", lambda e, pg=pg, sg_=sg_: e.activation(out=sg_.t[:, 0:NT], in_=pg.t[:, 0:NT], func=AF.Silu),
                             reads=[pg], writes=[sg_])
                        s.op("dve", lambda e, pu=pu, sg_=sg_, fc=fc, hd=hd: e.tensor_tensor(
                            out=hd.t[:, fc, :], in0=pu.t[:, 0:NT], in1=sg_.t[:, 0:NT], op=ALU.mult),
                            reads=[pu, sg_], writes=[hd])
                    q = 0
                    for sub in range(NSUB):
                        for n0 in range(0, D, 512):
                            ps = self.PS[4 + q % 4]
                            q += 1
                            for fc in range(fk):
                                s.op("pe", lambda e, fc=fc, sub=sub, n0=n0, ps=ps, wd=wd, hd=hd: e.matmul(
                                    ps.t[:, :], lhsT=hd.t[:, fc, sub * 128:(sub + 1) * 128],
                                    rhs=wd.t[:, fc * D + n0: fc * D + n0 + 512],
                                    start=(fc == 0), stop=(fc == fk - 1)), reads=[hd, wd], writes=[ps])
                            s.op("dve", lambda e, sub=sub, n0=n0, ps=ps, e_=e_: e.scalar_tensor_tensor(
                                out=xs[sub].t[:, n0:n0 + 512], in0=ps.t[:, :], scalar=coef[sub].t[:, e_:e_ + 1],
                                in1=xs[sub].t[:, n0:n0 + 512], op0=ALU.mult, op1=ALU.add),
                                reads=[ps, coef[sub], xs[sub]], writes=[xs[sub]])
                for sub in range(NSUB):
                    t0 = st * NT + sub * 128
                    s.dma("act", dst[t0:t0 + 128, :], xs[sub].t[:], reads=[xs[sub]])

    def _transpose32(self, h, hT32):
        s = self.s
        nk = self.c.KC
        g = 0
        for k0 in range(0, nk, 4):
            kn = min(4, nk - k0)
            ps = self.PS[4 + g % 4]
            g += 1
            for j in range(kn):
                s.op("pe", lambda e, j=j, k0=k0, ps=ps: e.transpose(ps.t[:, j * 128:(j + 1) * 128],
                                                                    h.t[:, (k0 + j) * 128:(k0 + j + 1) * 128], self.ident.t[:]),
                     reads=[h, self.ident], writes=[ps])
            s.op("dve", lambda e, ps=ps, k0=k0, kn=kn: e.tensor_copy(
                out=hT32.t[:, k0:k0 + kn, :], in_=ps.t[:, 0:kn * 128].rearrange("p (k t) -> p k t", t=128)),
                reads=[ps], writes=[hT32])

    def router(self, hT32, wr, brb, rt, coef):
        c, s = self.c, self.s
        KC = c.KC
        ps = self.PS[3]
        for kc in range(KC):
            s.op("pe", lambda e, kc=kc: e.matmul(ps.t[:, 0:36], lhsT=hT32.t[:, kc, :], rhs=wr.t[:, kc, :],
                                                  start=(kc == 0), stop=(kc == KC - 1)), reads=[hT32, wr], writes=[ps])
        R = rt.t
        V = lambda fn, rd=(rt,), wr_=(rt,): s.op("dve", fn, reads=list(rd), writes=list(wr_))
        V(lambda e: e.tensor_tensor(out=R[:, 0:36], in0=ps.t[:, 0:36], in1=brb.t[:, :], op=ALU.add), rd=(ps, brb))
        V(lambda e: e.tensor_reduce(out=R[:, 36:37], in_=R[:, 0:4], axis=AX.X, op=ALU.max))
        V(lambda e: e.tensor_scalar(out=R[:, 38:42], in0=R[:, 0:4], scalar1=R[:, 36:37], scalar2=None, op0=ALU.is_equal))
        V(lambda e: e.tensor_scalar(out=R[:, 42:46], in0=R[:, 0:4], scalar1=R[:, 36:37], scalar2=None, op0=ALU.subtract))
        s.op("act", lambda e: e.activation(out=R[:, 42:46], in_=R[:, 42:46], func=AF.Exp, accum_out=R[:, 37:38]),
             reads=[rt], writes=[rt])
        V(lambda e: e.reciprocal(out=R[:, 116:117], in_=R[:, 37:38]))
        V(lambda e: e.tensor_tensor(out=R[:, 48:80].rearrange("p (g j) -> p g j", g=4),
                                    in0=R[:, 4:36].rearrange("p (g j) -> p g j", g=4),
                                    in1=R[:, 38:42].rearrange("p (g o) -> p g o", o=1).to_broadcast([128, 4, 8])
                                    if hasattr(R[:, 38:42], "to_broadcast") else None, op=ALU.mult)) if False else None
        for g in range(4):
            if g == 0:
                V(lambda e: e.tensor_scalar(out=R[:, 80:88], in0=R[:, 4:12], scalar1=R[:, 38:39], scalar2=None, op0=ALU.mult))
            else:
                V(lambda e, g=g: e.scalar_tensor_tensor(out=R[:, 80:88], in0=R[:, 4 + 8 * g:12 + 8 * g], scalar=R[:, 38 + g:39 + g],
                                                        in1=R[:, 80:88], op0=ALU.mult, op1=ALU.add))
        V(lambda e: e.tensor_reduce(out=R[:, 88:89], in_=R[:, 80:88], axis=AX.X, op=ALU.max))
        V(lambda e: e.tensor_scalar(out=R[:, 90:98], in0=R[:, 80:88], scalar1=R[:, 88:89], scalar2=None, op0=ALU.is_equal))
        V(lambda e: e.scalar_tensor_tensor(out=R[:, 98:106], in0=R[:, 90:98], scalar=-1e30, in1=R[:, 80:88],
                                           op0=ALU.mult, op1=ALU.add))
        V(lambda e: e.tensor_reduce(out=R[:, 89:90], in_=R[:, 98:106], axis=AX.X, op=ALU.max))
        V(lambda e: e.tensor_scalar(out=R[:, 106:114], in0=R[:, 98:106], scalar1=R[:, 89:90], scalar2=None, op0=ALU.is_equal))
        V(lambda e: e.tensor_tensor(out=R[:, 114:115], in0=R[:, 88:89], in1=R[:, 89:90], op=ALU.subtract))
        s.op("act", lambda e: e.activation(out=R[:, 114:115], in_=R[:, 114:115], func=AF.Sigmoid), reads=[rt], writes=[rt])
        V(lambda e: e.tensor_scalar(out=R[:, 115:116], in0=R[:, 114:115], scalar1=-1.0, scalar2=1.0, op0=ALU.mult, op1=ALU.add))
        V(lambda e: e.tensor_tensor(out=R[:, 114:116], in0=R[:, 114:116], in1=R[:, 116:117].to_broadcast([128, 2])
                                    if False else R[:, 116:117], op=ALU.mult)) if False else None
        V(lambda e: e.tensor_scalar(out=R[:, 114:116], in0=R[:, 114:116], scalar1=R[:, 116:117], scalar2=None, op0=ALU.mult))
        V(lambda e: e.tensor_scalar(out=R[:, 120:128], in0=R[:, 90:98], scalar1=R[:, 114:115], scalar2=None, op0=ALU.mult))
        V(lambda e: e.scalar_tensor_tensor(out=R[:, 120:128], in0=R[:, 106:114], scalar=R[:, 115:116], in1=R[:, 120:128],
                                           op0=ALU.mult, op1=ALU.add))
        for g in range(4):
            s.op("dve", lambda e, g=g: e.tensor_scalar(out=coef.t[:, 8 * g:8 * g + 8], in0=R[:, 120:128], scalar1=R[:, 38 + g:39 + g],
                                                       scalar2=None, op0=ALU.mult), reads=[rt], writes=[coef])


def _t5_bucket(dist):
    n = np.maximum(dist, 0)
    nf = np.maximum(n, 1).astype(np.float32)
    large = 16 + (np.log(nf / 16) / math.log(128 / 16) * 16).astype(np.int32)
    large = np.minimum(large, 31)
    return np.where(n < 16, n, large)


def _bands():
    out = np.zeros((4, 3, 128, 128), np.float32)
    t = np.arange(128)[:, None]
    tp = np.arange(128)[None, :]
    for gi, w in enumerate((2, 4, 8, 16)):
        inw = ((tp - t) >= 0) & ((tp - t) <= w - 1)
        out[gi, 0] = np.where(inw, 1.0 / w, 0.0) - (t == tp)
        out[gi, 1] = np.where((tp + 128 - t) <= w - 1, 1.0 / w, 0.0)
        out[gi, 2] = np.where(inw, 1.0 / np.minimum(tp + 1, w), 0.0) - (t == tp)
    return out


def _biasT(rel_bias):
    j = np.arange(128)[:, None, None]
    blk = np.arange(2)[None, :, None]
    i = np.arange(128)[None, None, :]
    b = _t5_bucket(128 + i - (blk * 128 + j))
    t = np.asarray(rel_bias, np.float32)[b]
    return np.ascontiguousarray(np.transpose(t, (0, 3, 1, 2)).reshape(128, -1, 256))


def _maskT():
    j = np.arange(128)[:, None, None]
    blk = np.arange(2)[None, :, None]
    i = np.arange(128)[None, None, :]
    jj = blk * 128 + j
    return np.ascontiguousarray(((jj > i) & (jj <= i + 128)).astype(np.float32).reshape(128, 256))


def make_in_maps(cfg, inp):
    c = cfg
    D, KC = c.D, c.KC
    f = lambda a: np.ascontiguousarray(np.asarray(a, dtype=np.float32))
    bc = lambda a: f(np.broadcast_to(np.asarray(a)[..., None, :], a.shape[:-1] + (128, a.shape[-1])))
    L2 = c.DEPTH * 2
    shared = {
        "norm_g_bc": bc(inp["norm_g"].reshape(L2, D)),
        "ada_w": f(inp["ada_w"].reshape(L2, D, 3 * D)),
        "ada_b": f(inp["ada_b"].reshape(L2, 3 * D)),
        "ident": np.eye(128, dtype=np.float32),
        "ones": np.ones((128, 128), np.float32),
        "pool_w": f(inp["pool_w"]) if c.NP else np.zeros((1, 4, c.PG, c.PG), np.float32),
        "pool_scale_bc": bc(inp["pool_scale"]) if c.NP else np.zeros((1, 128, D), np.float32),
        "bands": _bands(),
        "attn_w_in": f(inp["attn_w_in"]),
        "attn_w_o": f(inp["attn_w_o"]),
        "attn_gq_bc": bc(inp["attn_q_gain"]),
        "attn_gk_bc": bc(inp["attn_k_gain"]),
        "attn_sinks_bc": bc(inp["attn_sinks"]),
        "biasT": _biasT(inp["rel_bias"]),
        "maskT": _maskT(),
        "rw_w_rkv": f(inp["rw_w_rkv"]), "rw_w1": f(inp["rw_w1"]), "rw_a1": f(inp["rw_a1"]), "rw_g1": f(inp["rw_g1"]),
        "rw_w2": f(inp["rw_w2"]), "rw_a2": f(inp["rw_a2"]), "rw_g2": f(inp["rw_g2"]), "rw_w_o": f(inp["rw_w_o"]),
        "rw_muT": f(np.transpose(np.asarray(inp["rw_mu"]).reshape(-1, 6, KC, 128), (0, 3, 1, 2))),
        "rw_w0_bc": bc(inp["rw_w0"]), "rw_a0_bc": bc(inp["rw_a0"]), "rw_k_k_bc": bc(inp["rw_k_k"]), "rw_k_a_bc": bc(inp["rw_k_a"]),
        "rw_r_k_bc": bc(np.asarray(inp["rw_r_k"]).reshape(-1, D)), "rw_lnx_g_bc": bc(inp["rw_lnx_g"]), "rw_lnx_b_bc": bc(inp["rw_lnx_b"]),
        "fsel": np.ascontiguousarray((np.arange(64 * 128)[None, :] // 64 == np.arange(128)[:, None]).astype(np.float32)),
        "moe_w_r": f(np.concatenate([inp["moe_w_grp"], inp["moe_w_exp"]], axis=-1)),
        "moe_b_r_bc": bc(np.concatenate([inp["moe_b_grp"], inp["moe_b_exp"]], axis=-1)),
        "moe_w_gate": f(inp["moe_w_gate"]),
        "moe_w_up": f(inp["moe_w_up"]),
        "moe_w_down": f(inp["moe_w_down"]),
    }
    maps = []
    for b in range(c.B):
        m = dict(shared)
        m["x"] = f(inp["x"][b])
        m["cT"] = f(np.asarray(inp["c"][b]).reshape(KC, 128).T)
        maps.append(m)
    return maps


def kernel(**inputs):
    cfg = Cfg()
    inp = {k: np.asarray(v) for k, v in inputs.items()}
    nc = Builder(cfg).build()
    maps = make_in_maps(cfg, inp)
    res = run_bass_kernel_spmd(nc, maps, core_ids=list(range(cfg.B)))
    return np.stack([r["y"] for r in res.results], axis=0).astype(np.float32)
```

```python
import contextlib
import math
import numpy as np
import concourse.bass as bass
import concourse.mybir as mybir
from concourse.bass_utils import run_bass_kernel_spmd

F32, BF16 = mybir.dt.float32, mybir.dt.bfloat16
AF = mybir.ActivationFunctionType
ALU = mybir.AluOpType
AX = mybir.AxisListType


class Cfg:
    def __init__(self, D=2048, S=8192, DEPTH=4, B=4):
        self.D, self.S, self.DEPTH, self.B = D, S, DEPTH, B
        self.KC = D // 128
        self.H = D // 64
        self.KV = max(1, self.H // 8)
        self.GQ = self.H // self.KV
        self.QKV = D + 2 * self.KV * 64
        self.LW = max(32, int(round(1.8 * D ** 0.5 / 32)) * 32)
        self.LA = self.LW
        self.LG = max(32, int(round(0.6 * D ** 0.8 / 32)) * 32)
        self.DE = D // 4
        self.PG = D // 4
        self.NE, self.NG, self.EPG = 32, 4, 8
        self.NA, self.NR, self.NP = (DEPTH + 2) // 3, (DEPTH + 1) // 3, DEPTH // 3
        self.NT = 512 if S >= 512 else S
        self.NSUB = self.NT // 128


class Buf:
    __slots__ = ("t", "w", "r")

    def __init__(self, t):
        self.t, self.w, self.r = t, {}, {}

    def __getitem__(self, k):
        return self.t[k]


class Sched:
    NDMA = 16

    def __init__(self, nc, es):
        self.nc = nc
        self.eng = dict(pe=nc.tensor, dve=nc.vector, act=nc.scalar, pool=nc.gpsimd, sp=nc.sync)
        self.sem, self.cnt, self.inc = {}, {}, {}
        for n in self.eng:
            self.sem[n] = es.enter_context(nc.semaphore("s_" + n))
            self.cnt[n], self.inc[n] = 0, 1
        for i in range(self.NDMA):
            n = "d%d" % i
            self.sem[n] = es.enter_context(nc.semaphore("s_" + n))
            self.cnt[n], self.inc[n] = 0, 16
        self.known = {e: {} for e in self.eng}
        self.di = 0

    def _waits(self, e, reads, writes):
        deps = {}
        for b in reads:
            for k, v in b.w.items():
                deps[k] = max(deps.get(k, 0), v)
        for b in writes:
            for k, v in b.w.items():
                deps[k] = max(deps.get(k, 0), v)
            for k, v in b.r.items():
                deps[k] = max(deps.get(k, 0), v)
        kn = self.known[e]
        for k, v in deps.items():
            if k == e and e == "pe":
                continue
            if kn.get(k, 0) < v:
                self.eng[e].wait_ge(self.sem[k], v)
                kn[k] = v

    def op(self, e, fn, reads=(), writes=()):
        self._waits(e, reads, writes)
        ins = fn(self.eng[e])
        self.cnt[e] += 1
        v = self.cnt[e]
        ins.then_inc(self.sem[e], 1)
        for b in reads:
            b.r[e] = v
        for b in writes:
            b.w = {e: v}
            b.r = {}

    def dma(self, q, out, in_, reads=(), writes=()):
        self._waits(q, reads, writes)
        n = "d%d" % self.di
        self.di = (self.di + 1) % self.NDMA
        ins = self.eng[q].dma_start(out=out, in_=in_)
        self.cnt[n] += 1
        v = self.cnt[n] * 16
        ins.then_inc(self.sem[n], 16)
        for b in reads:
            b.r[n] = v
        for b in writes:
            b.w = {n: v}
            b.r = {}

    def barrier(self):
        for e in self.eng:
            for k in self.sem:
                v = self.cnt[k] * self.inc[k]
                if v > 0 and k != e and self.known[e].get(k, 0) < v:
                    self.eng[e].wait_ge(self.sem[k], v)
                    self.known[e][k] = v


class Builder:
    def __init__(self, cfg):
        self.c = cfg
        self.nc = bass.Bass("TRN2", target_bir_lowering=False)
        self.ins = {}

    def inp(self, name, shape):
        self.ins[name] = self.nc.dram_tensor(name, list(shape), F32, kind="ExternalInput").ap()
        return self.ins[name]

    def sb(self, es, name, shape, dt=F32):
        self._uid = getattr(self, "_uid", 0) + 1
        return Buf(es.enter_context(self.nc.sbuf_tensor("sb%d_%s" % (self._uid, name), list(shape), dt)))

    def build(self):
        c, nc = self.c, self.nc
        D, S, KC = c.D, c.S, c.KC
        L2 = c.DEPTH * 2
        x_in = self.inp("x", [S, D])
        self.inp("cT", [128, KC])
        self.inp("norm_g_bc", [L2, 128, D])
        self.inp("ada_w", [L2, D, 3 * D])
        self.inp("ada_b", [L2, 3 * D])
        self.inp("ident", [128, 128])
        self.inp("ones", [128, 128])
        self.inp("pool_w", [max(c.NP, 1), 4, c.PG, c.PG])
        self.inp("pool_scale_bc", [max(c.NP, 1), 128, D])
        self.inp("bands", [4, 3, 128, 128])
        self.inp("attn_w_in", [c.NA, D, c.QKV])
        self.inp("attn_w_o", [c.NA, D, D])
        self.inp("attn_gq_bc", [c.NA, 128, 64])
        self.inp("attn_gk_bc", [c.NA, 128, 64])
        self.inp("attn_sinks_bc", [c.NA, 128, c.H])
        self.inp("biasT", [128, c.H, 256])
        self.inp("maskT", [128, 256])
        NRm = max(c.NR, 1)
        self.inp("rw_w_rkv", [NRm, 3, D, D])
        self.inp("rw_w1", [NRm, D, c.LW])
        self.inp("rw_a1", [NRm, D, c.LA])
        self.inp("rw_g1", [NRm, D, c.LG])
        self.inp("rw_w2", [NRm, c.LW, D])
        self.inp("rw_a2", [NRm, c.LA, D])
        self.inp("rw_g2", [NRm, c.LG, D])
        self.inp("rw_w_o", [NRm, D, D])
        self.inp("rw_muT", [NRm, 128, 6, KC])
        for nm in ("w0", "a0", "k_k", "k_a", "r_k", "lnx_g", "lnx_b"):
            self.inp("rw_%s_bc" % nm, [NRm, 128, D])
        self.inp("fsel", [128, 64 * 128])
        self.inp("moe_w_r", [c.DEPTH, D, 36])
        self.inp("moe_b_r_bc", [c.DEPTH, 128, 36])
        self.inp("moe_w_gate", [c.DEPTH, c.NE, D, c.DE])
        self.inp("moe_w_up", [c.DEPTH, c.NE, D, c.DE])
        self.inp("moe_w_down", [c.DEPTH, c.NE, c.DE, D])
        y = nc.dram_tensor("y", [S, D], F32, kind="ExternalOutput").ap()
        xa = nc.dram_tensor("xa", [S, D], F32, kind="Internal").ap()
        xb = nc.dram_tensor("xb", [S, D], F32, kind="Internal").ap()
        self.scr = {nm: nc.dram_tensor("scr_" + nm, [S, D], F32, kind="Internal").ap()
                    for nm in ("r", "w", "k", "v", "a", "b", "g", "y")}
        self.scr["bon"] = nc.dram_tensor("scr_bon", [S, c.H], F32, kind="Internal").ap()

        with contextlib.ExitStack() as es:
            self.s = Sched(nc, es)
            s = self.s
            self.PSW = [Buf(es.enter_context(nc.psum_tensor("psw%d" % i, [128, 1024], F32))) for i in range(4)]
            self.PS = [Buf(self.PSW[i // 2].t[:, (i % 2) * 512:(i % 2 + 1) * 512]) for i in range(8)]
            self.ident = self.sb(es, "ident", [128, 128])
            self.ones = self.sb(es, "ones", [128, 128])
            self.A = self.sb(es, "modA", [128, D])
            self.Bsh = self.sb(es, "modB", [128, D])
            self.G = self.sb(es, "modG", [128, D])
            self.sc = self.sb(es, "sc", [128, KC])
            s.dma("sp", self.ident.t[:], self.ins["ident"], writes=[self.ident])
            s.dma("sp", self.ones.t[:], self.ins["ones"], writes=[self.ones])
            s.dma("sp", self.sc.t[:], self.ins["cT"], writes=[self.sc])
            s.op("act", lambda e: e.activation(out=self.sc.t[:], in_=self.sc.t[:], func=AF.Silu),
                 reads=[self.sc], writes=[self.sc])
            cur = x_in
            nsub_layers = c.DEPTH * 2
            k = 0
            for layer in range(c.DEPTH):
                kind, idx = layer % 3, layer // 3
                for sub in range(2):
                    k += 1
                    dst = y if k == nsub_layers else (xa if k % 2 == 1 else xb)
                    self.phase_mod(layer * 2 + sub)
                    s.barrier()
                    if sub == 1:
                        self.phase_moe(layer, cur, dst)
                    elif kind == 2:
                        self.phase_pool(idx, cur, dst)
                    elif kind == 0:
                        self.phase_attn(idx, cur, dst)
                    elif kind == 1:
                        self.phase_rw_proj(idx, cur)
                        s.barrier()
                        self.phase_rw_scan()
                        s.barrier()
                        self.phase_rw_out(idx, cur, dst)
                    else:
                        self.phase_copy(cur, dst)
                    s.barrier()
                    cur = dst
            s.barrier()
        return nc

    def phase_mod(self, ls):
        c, nc, s = self.c, self.nc, self.s
        D, KC = c.D, c.KC
        N3 = 3 * D
        with contextlib.ExitStack() as es:
            wst = [self.sb(es, "mod_w%d" % i, [128, KC, 512]) for i in range(2)]
            row = self.sb(es, "mod_row", [1, N3])
            brow = self.sb(es, "mod_brow", [1, N3])
            gbc = self.sb(es, "mod_gbc", [128, D])
            s.dma("sp", brow.t[:], self.ins["ada_b"][ls:ls + 1, :], writes=[brow])
            s.dma("sp", gbc.t[:], self.ins["norm_g_bc"][ls], writes=[gbc])
            wv = self.ins["ada_w"][ls].rearrange("(kc p) n -> p kc n", p=128)
            nch = N3 // 512
            for n in range(nch):
                w = wst[n % 2]
                s.dma("sp", w.t[:], wv[:, :, n * 512:(n + 1) * 512], writes=[w])
                ps = self.PS[n % 2]
                for kc in range(KC):
                    s.op("pe", lambda e, kc=kc, w=w, ps=ps: e.matmul(ps.t[0:1, :], lhsT=self.sc.t[:, kc:kc + 1],
                                                                      rhs=w.t[:, kc, :], start=(kc == 0), stop=(kc == KC - 1)),
                         reads=[self.sc, w], writes=[ps])
                s.op("dve", lambda e, n=n, ps=ps: e.tensor_tensor(out=row.t[0:1, n * 512:(n + 1) * 512], in0=ps.t[0:1, :],
                                                                   in1=brow.t[0:1, n * 512:(n + 1) * 512], op=ALU.add),
                     reads=[ps, brow], writes=[row])
            for j in range(nch):
                ps = self.PS[2 + j % 2]
                s.op("pe", lambda e, j=j, ps=ps: e.matmul(ps.t[:, :], lhsT=self.ones.t[0:1, :], rhs=row.t[0:1, j * 512:(j + 1) * 512],
                                                           start=True, stop=True), reads=[self.ones, row], writes=[ps])
                sec, off = divmod(j * 512, D)
                if sec == 0:
                    s.op("act", lambda e, ps=ps, off=off: e.copy(out=self.Bsh.t[:, off:off + 512], in_=ps.t[:, :]),
                         reads=[ps], writes=[self.Bsh])
                elif sec == 1:
                    s.op("dve", lambda e, ps=ps, off=off: e.scalar_tensor_tensor(
                        out=self.A.t[:, off:off + 512], in0=ps.t[:, :], scalar=1.0, in1=gbc.t[:, off:off + 512],
                        op0=ALU.add, op1=ALU.mult), reads=[ps, gbc], writes=[self.A])
                else:
                    s.op("act", lambda e, ps=ps, off=off: e.copy(out=self.G.t[:, off:off + 512], in_=ps.t[:, :]),
                         reads=[ps], writes=[self.G])
            s.barrier()

    def norm_tile(self, xt, h, ss):
        c, s = self.c, self.s
        s.op("act", lambda e: e.activation(out=h.t[:], in_=xt.t[:], func=AF.Square, accum_out=ss.t[:, 0:1]),
             reads=[xt], writes=[h, ss])
        s.op("dve", lambda e: e.tensor_scalar(out=ss.t[:, 1:2], in0=ss.t[:, 0:1], scalar1=1.0 / c.D, scalar2=1e-6,
                                              op0=ALU.mult, op1=ALU.add), reads=[ss], writes=[ss])
        s.op("act", lambda e: e.activation(out=ss.t[:, 1:2], in_=ss.t[:, 1:2], func=AF.Sqrt), reads=[ss], writes=[ss])
        s.op("dve", lambda e: e.reciprocal(out=ss.t[:, 1:2], in_=ss.t[:, 1:2]), reads=[ss], writes=[ss])
        s.op("dve", lambda e: e.scalar_tensor_tensor(out=h.t[:], in0=xt.t[:], scalar=ss.t[:, 1:2], in1=self.A.t[:],
                                                     op0=ALU.mult, op1=ALU.mult), reads=[xt, ss, self.A], writes=[h])
        s.op("dve", lambda e: e.tensor_tensor(out=h.t[:], in0=h.t[:], in1=self.Bsh.t[:], op=ALU.add),
             reads=[h, self.Bsh], writes=[h])

    def transpose_to(self, src, ncols, dsts, tok0, psums):
        s = self.s
        nk = ncols // 128
        g = 0
        for k0 in range(0, nk, 4):
            kn = min(4, nk - k0)
            ps = psums[g % len(psums)]
            g += 1
            for j in range(kn):
                s.op("pe", lambda e, j=j, k0=k0, ps=ps: e.transpose(ps.t[:, j * 128:(j + 1) * 128],
                                                                    src.t[:, (k0 + j) * 128:(k0 + j + 1) * 128], self.ident.t[:]),
                     reads=[src, self.ident], writes=[ps])
            for di, dst in enumerate(dsts):
                eng = "act" if di == 0 else "dve"
                if eng == "act":
                    s.op("act", lambda e, dst=dst, ps=ps, k0=k0, kn=kn: e.copy(
                        out=dst.t[:, k0:k0 + kn, tok0:tok0 + 128],
                        in_=ps.t[:, 0:kn * 128].rearrange("p (k t) -> p k t", t=128)), reads=[ps], writes=[dst])
                else:
                    s.op("dve", lambda e, dst=dst, ps=ps, k0=k0, kn=kn: e.tensor_copy(
                        out=dst.t[:, k0:k0 + kn, tok0:tok0 + 128],
                        in_=ps.t[:, 0:kn * 128].rearrange("p (k t) -> p k t", t=128)), reads=[ps], writes=[dst])

    def load_w(self, wap, K, N, wsts, wbf, scale_bc=None):
        s = self.s
        kcs = (K + 127) // 128
        rows = min(128, K)
        cap = wsts[0].t.shape[1]
        kper = max(1, min(kcs, cap // N))
        parts = [(k0, min(kcs, k0 + kper)) for k0 in range(0, kcs, kper)]
        for (k0, k1) in parts:
            self._wsti = getattr(self, "_wsti", 0) + 1
            wst = wsts[self._wsti % 2]
            n = (k1 - k0) * N
            if K >= 128:
                src = wap.rearrange("(kc p) n -> p kc n", p=128)[:, k0:k1, :]
                s.dma("sp", wst.t[:, 0:n].rearrange("p (kc n) -> p kc n", n=N), src, writes=[wst])
            else:
                s.dma("sp", wst.t[0:rows, 0:N], wap, writes=[wst])
            if scale_bc is None:
                s.op("act", lambda e, wst=wst, n=n, k0=k0: e.copy(out=wbf.t[0:rows, k0 * N:k0 * N + n], in_=wst.t[0:rows, 0:n]),
                     reads=[wst], writes=[wbf])
            else:
                for kc in range(k0, k1):
                    s.op("dve", lambda e, kc=kc, wst=wst, k0=k0: e.tensor_tensor(
                        out=wbf.t[0:rows, kc * N:(kc + 1) * N], in0=wst.t[0:rows, (kc - k0) * N:(kc - k0 + 1) * N],
                        in1=scale_bc.t[0:rows, 0:N], op=ALU.mult), reads=[wst, scale_bc], writes=[wbf])

    def phase_copy(self, src, dst):
        c, s = self.c, self.s
        with contextlib.ExitStack() as es:
            t = [self.sb(es, "cp%d" % i, [128, c.D]) for i in range(2)]
            for i in range(c.S // 128):
                b = t[i % 2]
                s.dma("sp", b.t[:], src[i * 128:(i + 1) * 128, :], writes=[b])
                s.dma("act", dst[i * 128:(i + 1) * 128, :], b.t[:], reads=[b])

    def phase_pool(self, idx, src, dst):
        c, s = self.c, self.s
        D, KC, PG = c.D, c.KC, c.PG
        pk = PG // 128
        with contextlib.ExitStack() as es:
            xt = [self.sb(es, "pl_x%d" % i, [128, D]) for i in range(2)]
            h = self.sb(es, "pl_h", [128, D])
            hb = [self.sb(es, "pl_hb%d" % i, [128, D], BF16) for i in range(2)]
            ss = self.sb(es, "pl_ss", [128, 2])
            bst = self.sb(es, "pl_bst", [128, 12, 128])
            bands = self.sb(es, "pl_bands", [128, 12, 128], BF16)
            wst = self.sb(es, "pl_wst", [128, 4 * pk * PG])
            wbf = self.sb(es, "pl_wbf", [128, 4 * pk * PG], BF16)
            psc = self.sb(es, "pl_psc", [128, D])
            pT = [self.sb(es, "pl_pT%d" % i, [128, KC, 128], BF16) for i in range(2)]
            s.dma("sp", bst.t[:], self.ins["bands"].rearrange("w k p t -> p (w k) t"), writes=[bst])
            s.op("pool", lambda e: e.tensor_copy(out=bands.t[:], in_=bst.t[:]), reads=[bst], writes=[bands])
            s.dma("sp", psc.t[:], self.ins["pool_scale_bc"][idx], writes=[psc])
            s.dma("sp", wst.t[:, :].rearrange("p (g kc n) -> p g kc n", g=4, kc=pk),
                  self.ins["pool_w"][idx].rearrange("g (kc p) n -> p g kc n", p=128), writes=[wst])
            s.op("pool", lambda e: e.tensor_copy(out=wbf.t[:], in_=wst.t[:]), reads=[wst], writes=[wbf])
            s.op("dve", lambda e: e.tensor_tensor(out=psc.t[:], in0=psc.t[:], in1=self.G.t[:], op=ALU.mult),
                 reads=[psc, self.G], writes=[psc])
            for i in range(c.S // 128):
                x_ = xt[i % 2]
                hcur, hprev = hb[i % 2], hb[(i + 1) % 2]
                s.dma("sp", x_.t[:], src[i * 128:(i + 1) * 128, :], writes=[x_])
                self.norm_tile(x_, h, ss)
                s.op("act", lambda e, hcur=hcur: e.copy(out=hcur.t[:], in_=h.t[:]), reads=[h], writes=[hcur])
                p_ = pT[i % 2]
                for kc in range(KC):
                    g = (kc * 128) // PG
                    ps = self.PS[kc % 4]
                    bcur = g * 3 + (2 if i == 0 else 0)
                    s.op("pe", lambda e, kc=kc, ps=ps, bcur=bcur: e.matmul(
                        ps.t[:, 0:128], lhsT=hcur.t[:, kc * 128:(kc + 1) * 128], rhs=bands.t[:, bcur, :],
                        start=True, stop=(i == 0)), reads=[hcur, bands], writes=[ps])
                    if i > 0:
                        s.op("pe", lambda e, kc=kc, ps=ps, g=g: e.matmul(
                            ps.t[:, 0:128], lhsT=hprev.t[:, kc * 128:(kc + 1) * 128], rhs=bands.t[:, g * 3 + 1, :],
                            start=False, stop=True), reads=[hprev, bands], writes=[ps])
                    s.op("act", lambda e, kc=kc, ps=ps: e.copy(out=p_.t[:, kc, :], in_=ps.t[:, 0:128]),
                         reads=[ps], writes=[p_])
                for g in range(4):
                    ps = self.PS[4 + g % 4]
                    for kc in range(pk):
                        s.op("pe", lambda e, g=g, kc=kc, ps=ps: e.matmul(
                            ps.t[:, 0:PG], lhsT=p_.t[:, g * pk + kc, :],
                            rhs=wbf.t[:, (g * pk + kc) * PG:(g * pk + kc + 1) * PG],
                            start=(kc == 0), stop=(kc == pk - 1)), reads=[p_, wbf], writes=[ps])
                    s.op("dve", lambda e, g=g, ps=ps: e.tensor_tensor(out=h.t[:, g * PG:(g + 1) * PG], in0=ps.t[:, 0:PG],
                                                                      in1=psc.t[:, g * PG:(g + 1) * PG], op=ALU.mult),
                         reads=[ps, psc], writes=[h])
                s.op("dve", lambda e: e.tensor_tensor(out=x_.t[:], in0=x_.t[:], in1=h.t[:], op=ALU.add),
                     reads=[x_, h], writes=[x_])
                s.dma("act", dst[i * 128:(i + 1) * 128, :], x_.t[:], reads=[x_])

    def phase_attn(self, idx, src, dst):
        c, s = self.c, self.s
        D, KC, H, KV, GQ, QKV = c.D, c.KC, c.H, c.KV, c.GQ, c.QKV
        NTA = min(256, c.S)
        NS = NTA // 128
        QK = D + KV * 64
        with contextlib.ExitStack() as es:
            xs = [self.sb(es, "at_x%d" % i, [128, D]) for i in range(NS)]
            h = self.sb(es, "at_h", [128, D])
            ss = self.sb(es, "at_ss", [128, 2])
            hT = self.sb(es, "at_hT", [128, KC, NTA], BF16)
            oT = self.sb(es, "at_oT", [128, KC, NTA], BF16)
            qkv = [self.sb(es, "at_qkv%d" % i, [128, QKV]) for i in range(NS)]
            wst = [self.sb(es, "at_wst%d" % i, [128, KC * 256]) for i in range(2)]
            wbf = [self.sb(es, "at_wbf%d" % i, [128, KC * 512], BF16) for i in range(2)]
            biasT = self.sb(es, "at_bias", [128, H, 256])
            mask = self.sb(es, "at_mask", [128, 256])
            qn = self.sb(es, "at_qn", [128, QK])
            ssq = self.sb(es, "at_ssq", [128, H + KV])
            qT = self.sb(es, "at_qT", [64, H, 128], BF16)
            kT = [self.sb(es, "at_kT%d" % i, [64, KV, 128], BF16) for i in range(2)]
            vx = [self.sb(es, "at_vx%d" % i, [128, KV, 65], BF16) for i in range(2)]
            ssb = [self.sb(es, "at_ssb%d" % i, [128, 256]) for i in range(1)] * 2
            pf = [self.sb(es, "at_pf%d" % i, [128, 256]) for i in range(1)] * 2
            pT = [self.sb(es, "at_pT%d" % i, [128, 256], BF16) for i in range(2)]
            esink = self.sb(es, "at_esink", [128, H])
            gqk = self.sb(es, "at_gqk", [128, 64])
            gk_ = self.sb(es, "at_gk", [128, 64])
            den = self.sb(es, "at_den", [128, 2])
            s.dma("sp", biasT.t[:], self.ins["biasT"], writes=[biasT])
            s.dma("sp", mask.t[:], self.ins["maskT"], writes=[mask])
            s.dma("sp", esink.t[:], self.ins["attn_sinks_bc"][idx], writes=[esink])
            s.dma("sp", gqk.t[:], self.ins["attn_gq_bc"][idx], writes=[gqk])
            s.dma("sp", gk_.t[:], self.ins["attn_gk_bc"][idx], writes=[gk_])
            s.op("act", lambda e: e.activation(out=esink.t[:], in_=esink.t[:], func=AF.Exp), reads=[esink], writes=[esink])
            s.op("dve", lambda e: e.scalar_tensor_tensor(out=gqk.t[:], in0=gqk.t[:], scalar=0.125, in1=gk_.t[:],
                                                         op0=ALU.mult, op1=ALU.mult), reads=[gqk, gk_], writes=[gqk])
            for i in range(2):
                s.op("pool", lambda e, i=i: e.memset(vx[i].t[:], 1.0), writes=[vx[i]])
            w_in, w_o = self.ins["attn_w_in"][idx], self.ins["attn_w_o"][idx]
            blk = 0
            wi = 0
            for st in range(c.S // NTA):
                for sub in range(NS):
                    t0 = st * NTA + sub * 128
                    s.dma("sp", xs[sub].t[:], src[t0:t0 + 128, :], writes=[xs[sub]])
                    self.norm_tile(xs[sub], h, ss)
                    self.transpose_to(h, D, [hT], sub * 128, self.PS[4:8])
                q = 0
                for n0 in range(0, QKV, 512):
                    nw = min(512, QKV - n0)
                    wb = wbf[wi % 2]
                    wi += 1
                    self.load_w(w_in[:, n0:n0 + nw], D, nw, wst, wb)
                    for sub in range(NS):
                        ps = self.PS[q % 4]
                        q += 1
                        for kc in range(KC):
                            s.op("pe", lambda e, kc=kc, sub=sub, ps=ps, wb=wb, nw=nw: e.matmul(
                                ps.t[:, 0:nw], lhsT=hT.t[:, kc, sub * 128:(sub + 1) * 128], rhs=wb.t[:, kc * nw:(kc + 1) * nw],
                                start=(kc == 0), stop=(kc == KC - 1)), reads=[hT, wb], writes=[ps])
                        s.op("act", lambda e, sub=sub, ps=ps, n0=n0, nw=nw: e.copy(out=qkv[sub].t[:, n0:n0 + nw], in_=ps.t[:, 0:nw]),
                             reads=[ps], writes=[qkv[sub]])
                for sub in range(NS):
                    first = (blk == 0)
                    kc_, kp_ = kT[blk % 2], kT[(blk + 1) % 2]
                    vc_, vp_ = vx[blk % 2], vx[(blk + 1) % 2]
                    blk += 1
                    qk = qkv[sub]
                    s.op("dve", lambda e: e.tensor_tensor(out=qn.t[:, 0:QK], in0=qk.t[:, 0:QK], in1=qk.t[:, 0:QK], op=ALU.mult),
                         reads=[qk], writes=[qn])
                    s.op("dve", lambda e: e.tensor_reduce(out=ssq.t[:, :], in_=qn.t[:, 0:QK].rearrange("p (h d) -> p h d", d=64),
                                                          axis=AX.X, op=ALU.add), reads=[qn], writes=[ssq])
                    s.op("dve", lambda e: e.tensor_scalar(out=ssq.t[:, :], in0=ssq.t[:, :], scalar1=1.0 / 64, scalar2=1e-6,
                                                          op0=ALU.mult, op1=ALU.add), reads=[ssq], writes=[ssq])
                    s.op("act", lambda e: e.activation(out=ssq.t[:, :], in_=ssq.t[:, :], func=AF.Sqrt), reads=[ssq], writes=[ssq])
                    s.op("dve", lambda e: e.reciprocal(out=ssq.t[:, :], in_=ssq.t[:, :]), reads=[ssq], writes=[ssq])
                    for hh in range(H):
                        s.op("dve", lambda e, hh=hh: e.tensor_scalar(out=qn.t[:, hh * 64:(hh + 1) * 64], in0=qk.t[:, hh * 64:(hh + 1) * 64],
                                                                      scalar1=ssq.t[:, hh:hh + 1], scalar2=None, op0=ALU.mult),
                             reads=[qk, ssq], writes=[qn])
                    for kv in range(KV):
                        s.op("dve", lambda e, kv=kv: e.scalar_tensor_tensor(
                            out=qn.t[:, D + kv * 64:D + (kv + 1) * 64], in0=qk.t[:, D + kv * 64:D + (kv + 1) * 64],
                            scalar=ssq.t[:, H + kv:H + kv + 1], in1=gqk.t[:, :], op0=ALU.mult, op1=ALU.mult),
                            reads=[qk, ssq, gqk], writes=[qn])
                        s.op("act", lambda e, kv=kv: e.copy(out=vc_.t[:, kv, 0:64], in_=qk.t[:, QK + kv * 64:QK + (kv + 1) * 64]),
                             reads=[qk], writes=[vc_])
                    g = 0
                    for h0 in range(0, H + KV, 4):
                        hn = min(4, H + KV - h0)
                        ps = self.PS[g % 4]
                        g += 1
                        for j in range(hn):
                            s.op("pe", lambda e, j=j, h0=h0, ps=ps: e.transpose(ps.t[0:64, j * 128:(j + 1) * 128],
                                                                                qn.t[:, (h0 + j) * 64:(h0 + j + 1) * 64], self.ident.t[:]),
                                 reads=[qn, self.ident], writes=[ps])
                        for j in range(hn):
                            hh = h0 + j
                            dstv = qT.t[:, hh, :] if hh < H else kc_.t[:, hh - H, :]
                            dbuf = qT if hh < H else kc_
                            eng = "act" if j % 2 == 0 else "dve"
                            if eng == "act":
                                s.op("act", lambda e, j=j, ps=ps, dstv=dstv: e.copy(out=dstv, in_=ps.t[0:64, j * 128:(j + 1) * 128]),
                                     reads=[ps], writes=[dbuf])
                            else:
                                s.op("dve", lambda e, j=j, ps=ps, dstv=dstv: e.tensor_copy(out=dstv, in_=ps.t[0:64, j * 128:(j + 1) * 128]),
                                     reads=[ps], writes=[dbuf])
                    c0 = 128 if first else 0
                    for hh in range(H):
                        kv = hh // GQ
                        ps = self.PS[hh % 2]
                        po = self.PS[2 + (hh // 4) % 2]
                        j = hh % 4
                        if not first:
                            s.op("pe", lambda e, hh=hh, kv=kv, ps=ps: e.matmul(ps.t[:, 0:128], lhsT=kp_.t[:, kv, :], rhs=qT.t[:, hh, :],
                                                                               start=True, stop=True), reads=[kp_, qT], writes=[ps])
                        s.op("pe", lambda e, hh=hh, kv=kv, ps=ps: e.matmul(ps.t[:, 128:256], lhsT=kc_.t[:, kv, :], rhs=qT.t[:, hh, :],
                                                                           start=True, stop=True), reads=[kc_, qT], writes=[ps])
                        sb_, pf_, pT_ = ssb[hh % 2], pf[hh % 2], pT[hh % 2]
                        s.op("dve", lambda e, hh=hh, ps=ps, sb_=sb_: e.tensor_tensor(out=sb_.t[:, c0:256], in0=ps.t[:, c0:256],
                                                                                     in1=biasT.t[:, hh, c0:256], op=ALU.add),
                             reads=[ps, biasT], writes=[sb_])
                        s.op("act", lambda e, sb_=sb_, pf_=pf_: e.activation(out=pf_.t[:, c0:256], in_=sb_.t[:, c0:256], func=AF.Exp),
                             reads=[sb_], writes=[pf_])
                        s.op("pool", lambda e, pf_=pf_, pT_=pT_: e.tensor_tensor(out=pT_.t[:, c0:256], in0=pf_.t[:, c0:256],
                                                                                 in1=mask.t[:, c0:256], op=ALU.mult),
                             reads=[pf_, mask], writes=[pT_])
                        if not first:
                            s.op("pe", lambda e, kv=kv, po=po, j=j, pT_=pT_: e.matmul(po.t[:, j * 128:j * 128 + 65], lhsT=pT_.t[:, 0:128],
                                                                                      rhs=vp_.t[:, kv, :], start=True, stop=False),
                                 reads=[pT_, vp_], writes=[po])
                        s.op("pe", lambda e, kv=kv, po=po, j=j, pT_=pT_: e.matmul(po.t[:, j * 128:j * 128 + 65], lhsT=pT_.t[:, 128:256],
                                                                                  rhs=vc_.t[:, kv, :], start=first, stop=True),
                             reads=[pT_, vc_], writes=[po])
                        s.op("dve", lambda e, hh=hh, po=po, j=j: e.tensor_tensor(out=den.t[:, 0:1], in0=po.t[:, j * 128 + 64:j * 128 + 65],
                                                                                 in1=esink.t[:, hh:hh + 1], op=ALU.add),
                             reads=[po, esink], writes=[den])
                        s.op("dve", lambda e: e.reciprocal(out=den.t[:, 1:2], in_=den.t[:, 0:1]), reads=[den], writes=[den])
                        s.op("dve", lambda e, hh=hh, po=po, j=j: e.tensor_scalar(out=h.t[:, hh * 64:(hh + 1) * 64], in0=po.t[:, j * 128:j * 128 + 64],
                                                                                 scalar1=den.t[:, 1:2], scalar2=None, op0=ALU.mult),
                             reads=[po, den], writes=[h])
                    self.transpose_to(h, D, [oT], sub * 128, self.PS[4:8])
                q = 0
                for n0 in range(0, D, 512):
                    wb = wbf[wi % 2]
                    wi += 1
                    self.load_w(w_o[:, n0:n0 + 512], D, 512, wst, wb, scale_bc=None)
                    for sub in range(NS):
                        ps = self.PS[q % 4]
                        q += 1
                        for kc in range(KC):
                            s.op("pe", lambda e, kc=kc, sub=sub, ps=ps, wb=wb: e.matmul(
                                ps.t[:, :], lhsT=oT.t[:, kc, sub * 128:(sub + 1) * 128], rhs=wb.t[:, kc * 512:(kc + 1) * 512],
                                start=(kc == 0), stop=(kc == KC - 1)), reads=[oT, wb], writes=[ps])
                        s.op("dve", lambda e, sub=sub, ps=ps, n0=n0: e.tensor_tensor(out=h.t[:, 0:512], in0=ps.t[:, :],
                                                                                     in1=self.G.t[:, n0:n0 + 512], op=ALU.mult),
                             reads=[ps, self.G], writes=[h])
                        s.op("dve", lambda e, sub=sub, n0=n0: e.tensor_tensor(out=xs[sub].t[:, n0:n0 + 512], in0=xs[sub].t[:, n0:n0 + 512],
                                                                              in1=h.t[:, 0:512], op=ALU.add),
                             reads=[h, xs[sub]], writes=[xs[sub]])
                for sub in range(NS):
                    t0 = st * NTA + sub * 128
                    s.dma("act", dst[t0:t0 + 128, :], xs[sub].t[:], reads=[xs[sub]])

    def phase_rw_proj(self, idx, src):
        c, s = self.c, self.s
        D, KC, H, LW, LA, LG = c.D, c.KC, c.H, c.LW, c.LA, c.LG
        LGC = (LG + 127) // 128
        lgr = min(128, LG)
        I = self.ins
        perm = lambda ap: ap.rearrange("t (hh j k) -> t j hh k", hh=2, k=64)
        with contextlib.ExitStack() as es:
            x_ = self.sb(es, "rp_x", [128, D])
            h = self.sb(es, "rp_h", [128, D])
            ss = self.sb(es, "rp_ss", [128, 2])
            hT = self.sb(es, "rp_hT", [128, KC, 129])
            xx = self.sb(es, "rp_xx", [128, KC, 128])
            xm = [self.sb(es, "rp_xm%d" % i, [128, KC, 128], BF16) for i in range(4)]
            wst = [self.sb(es, "rp_wst%d" % i, [128, max(KC * 128, D)]) for i in range(2)]
            wbf = [self.sb(es, "rp_wbf%d" % i, [128, KC * 512], BF16) for i in range(3)]
            w1b = self.sb(es, "rp_w1b", [128, KC * LW], BF16)
            a1b = self.sb(es, "rp_a1b", [128, KC * LA], BF16)
            g1b = self.sb(es, "rp_g1b", [128, KC * LG], BF16)
            w2b = self.sb(es, "rp_w2b", [128, D], BF16)
            a2b = self.sb(es, "rp_a2b", [128, D], BF16)
            g2b = self.sb(es, "rp_g2b", [128, LGC * D], BF16)
            muT = self.sb(es, "rp_muT", [128, 6, KC])
            twT = self.sb(es, "rp_twT", [128, 128], BF16)
            taT = self.sb(es, "rp_taT", [128, 128], BF16)
            tgT = self.sb(es, "rp_tgT", [128, LGC, 128], BF16)
            bcn = ("w0", "a0", "k_k", "k_a", "r_k")
            bcc = {nm: self.sb(es, "rp_bc_" + nm, [128, 512]) for nm in bcn}
            E = {nm: self.sb(es, "rp_e_" + nm, [128, 512]) for nm in
                 ("r", "v", "g", "zw", "a", "k", "kk", "kkn", "bs", "k2", "u")}
            E["sq"] = E["u"]
            E["t"] = E["u"]
            E["as"] = E["kk"]
            sm = self.sb(es, "rp_sm", [128, 16])
            bon = self.sb(es, "rp_bon", [128, H])
            wstbig = wst[0]
            for (nm, dstb, K_, N_) in (("rw_w1", w1b, D, LW), ("rw_a1", a1b, D, LA), ("rw_g1", g1b, D, LG),
                                       ("rw_w2", w2b, LW, D), ("rw_a2", a2b, LA, D)):
                self.load_w(I[nm][idx], K_, N_, wst, dstb)
            for lc in range(LGC):
                r_ = min(128, LG - lc * 128)
                s.dma("sp", wstbig.t[0:r_, 0:D], I["rw_g2"][idx][lc * 128:lc * 128 + r_, :], writes=[wstbig])
                s.op("pool", lambda e, lc=lc, r_=r_: e.tensor_copy(out=g2b.t[0:r_, lc * D:(lc + 1) * D], in_=wstbig.t[0:r_, 0:D]),
                     reads=[wstbig], writes=[g2b])
            s.dma("sp", muT.t[:], I["rw_muT"][idx], writes=[muT])
            s.op("dve", lambda e: e.memset(hT.t[:, :, 0:1], 0.0), writes=[hT])
            wr_ = I["rw_w_rkv"][idx]
            for i in range(c.S // 128):
                t0 = i * 128
                s.dma("sp", x_.t[:], src[t0:t0 + 128, :], writes=[x_])
                self.norm_tile(x_, h, ss)
                g = 0
                for k0 in range(0, KC, 4):
                    kn = min(4, KC - k0)
                    ps = self.PS[4 + g % 4]
                    g += 1
                    for j in range(kn):
                        s.op("pe", lambda e, j=j, k0=k0, ps=ps: e.transpose(ps.t[:, j * 128:(j + 1) * 128],
                                                                            h.t[:, (k0 + j) * 128:(k0 + j + 1) * 128], self.ident.t[:]),
                             reads=[h, self.ident], writes=[ps])
                    s.op("act", lambda e, ps=ps, k0=k0, kn=kn: e.copy(
                        out=hT.t[:, k0:k0 + kn, 1:129], in_=ps.t[:, 0:kn * 128].rearrange("p (k t) -> p k t", t=128)),
                        reads=[ps], writes=[hT])
                s.op("dve", lambda e: e.tensor_tensor(out=xx.t[:], in0=hT.t[:, :, 0:128], in1=hT.t[:, :, 1:129], op=ALU.subtract),
                     reads=[hT], writes=[xx])

                def mix(m, dstb):
                    for kc in range(KC):
                        s.op("dve", lambda e, kc=kc: e.scalar_tensor_tensor(
                            out=dstb.t[:, kc, :], in0=xx.t[:, kc, :], scalar=muT.t[:, m, kc:kc + 1], in1=hT.t[:, kc, 1:129],
                            op0=ALU.mult, op1=ALU.add), reads=[xx, muT, hT], writes=[dstb])

                mix(3, xm[3])
                ps = self.PS[0]
                for kc in range(KC):
                    s.op("pe", lambda e, kc=kc: e.matmul(ps.t[0:LW, 0:128], lhsT=w1b.t[:, kc * LW:(kc + 1) * LW], rhs=xm[3].t[:, kc, :],
                                                          start=(kc == 0), stop=(kc == KC - 1)), reads=[w1b, xm[3]], writes=[ps])
                s.op("act", lambda e: e.activation(out=twT.t[0:LW, :], in_=ps.t[0:LW, 0:128], func=AF.Tanh), reads=[ps], writes=[twT])
                mix(4, xm[3])
                ps = self.PS[1]
                for kc in range(KC):
                    s.op("pe", lambda e, kc=kc: e.matmul(ps.t[0:LA, 0:128], lhsT=a1b.t[:, kc * LA:(kc + 1) * LA], rhs=xm[3].t[:, kc, :],
                                                          start=(kc == 0), stop=(kc == KC - 1)), reads=[a1b, xm[3]], writes=[ps])
                s.op("act", lambda e: e.copy(out=taT.t[0:LA, :], in_=ps.t[0:LA, 0:128]), reads=[ps], writes=[taT])
                mix(5, xm[3])
                for lc in range(LGC):
                    r_ = min(128, LG - lc * 128)
                    ps = self.PS[2 + lc % 2]
                    for kc in range(KC):
                        s.op("pe", lambda e, kc=kc, lc=lc, r_=r_, ps=ps: e.matmul(
                            ps.t[0:r_, 0:128], lhsT=g1b.t[:, kc * LG + lc * 128: kc * LG + lc * 128 + r_], rhs=xm[3].t[:, kc, :],
                            start=(kc == 0), stop=(kc == KC - 1)), reads=[g1b, xm[3]], writes=[ps])
                    s.op("act", lambda e, lc=lc, r_=r_, ps=ps: e.activation(out=tgT.t[0:r_, lc, :], in_=ps.t[0:r_, 0:128], func=AF.Sigmoid),
                         reads=[ps], writes=[tgT])
                for m in range(3):
                    mix(m, xm[m])
                for n0 in range(0, D, 512):
                    sl = slice(n0, n0 + 512)
                    for m in range(3):
                        self.load_w(wr_[m][:, sl], D, 512, wst, wbf[m])
                    for nm in bcn:
                        s.dma("sp", bcc[nm].t[:], I["rw_%s_bc" % nm][idx][:, sl], writes=[bcc[nm]])
                    Pr, Pk, Pv, Pw, Pa, Pg = (self.PS[j] for j in range(6))
                    for m, P_ in ((0, Pr), (1, Pk), (2, Pv)):
                        for kc in range(KC):
                            s.op("pe", lambda e, kc=kc, m=m, P_=P_: e.matmul(P_.t[:, :], lhsT=xm[m].t[:, kc, :],
                                                                             rhs=wbf[m].t[:, kc * 512:(kc + 1) * 512],
                                                                             start=(kc == 0), stop=(kc == KC - 1)),
                                 reads=[xm[m], wbf[m]], writes=[P_])
                    s.op("pe", lambda e: e.matmul(Pw.t[:, :], lhsT=twT.t[0:LW, :], rhs=w2b.t[0:LW, sl], start=True, stop=True),
                         reads=[twT, w2b], writes=[Pw])
                    s.op("pe", lambda e: e.matmul(Pa.t[:, :], lhsT=taT.t[0:LA, :], rhs=a2b.t[0:LA, sl], start=True, stop=True),
                         reads=[taT, a2b], writes=[Pa])
                    for lc in range(LGC):
                        r_ = min(128, LG - lc * 128)
                        s.op("pe", lambda e, lc=lc, r_=r_: e.matmul(Pg.t[:, :], lhsT=tgT.t[0:r_, lc, :],
                                                                    rhs=g2b.t[0:r_, lc * D + n0: lc * D + n0 + 512],
                                                                    start=(lc == 0), stop=(lc == LGC - 1)), reads=[tgT, g2b], writes=[Pg])
                    A_ = lambda fn, rd, wr: s.op("act", fn, reads=rd, writes=wr)
                    V_ = lambda fn, rd, wr: s.op("dve", fn, reads=rd, writes=wr)
                    G_ = lambda fn, rd, wr: s.op("pool", fn, reads=rd, writes=wr)
                    j0 = n0 // 128
                    def pstore(nm, b):
                        for hh in range(2):
                            s.dma("act", perm(self.scr[nm][t0:t0 + 128, :])[:, j0:j0 + 4, hh, :],
                                  b.t[:, :].rearrange("p (j hh k) -> p j hh k", hh=2, k=64)[:, :, hh, :], reads=[b])
                    A_(lambda e: e.copy(out=E["r"].t[:], in_=Pr.t[:, :]), [Pr], [E["r"]])
                    pstore("r", E["r"])
                    A_(lambda e: e.copy(out=E["v"].t[:], in_=Pv.t[:, :]), [Pv], [E["v"]])
                    s.dma("act", self.scr["v"][t0:t0 + 128, sl], E["v"].t[:], reads=[E["v"]])
                    A_(lambda e: e.copy(out=E["g"].t[:], in_=Pg.t[:, :]), [Pg], [E["g"]])
                    s.dma("act", self.scr["g"][t0:t0 + 128, sl], E["g"].t[:], reads=[E["g"]])
                    V_(lambda e: e.tensor_tensor(out=E["zw"].t[:], in0=Pw.t[:, :], in1=bcc["w0"].t[:], op=ALU.add), [Pw, bcc["w0"]], [E["zw"]])
                    A_(lambda e: e.activation(out=E["zw"].t[:], in_=E["zw"].t[:], func=AF.Sigmoid), [E["zw"]], [E["zw"]])
                    A_(lambda e: e.activation(out=E["zw"].t[:], in_=E["zw"].t[:], func=AF.Exp, scale=-math.exp(-0.5)), [E["zw"]], [E["zw"]])
                    pstore("w", E["zw"])
                    V_(lambda e: e.tensor_tensor(out=E["a"].t[:], in0=Pa.t[:, :], in1=bcc["a0"].t[:], op=ALU.add), [Pa, bcc["a0"]], [E["a"]])
                    A_(lambda e: e.activation(out=E["a"].t[:], in_=E["a"].t[:], func=AF.Sigmoid), [E["a"]], [E["a"]])
                    A_(lambda e: e.copy(out=E["k"].t[:], in_=Pk.t[:, :]), [Pk], [E["k"]])
                    G_(lambda e: e.tensor_tensor(out=E["kk"].t[:], in0=E["k"].t[:], in1=bcc["k_k"].t[:], op=ALU.mult), [E["k"], bcc["k_k"]], [E["kk"]])
                    G_(lambda e: e.tensor_tensor(out=E["sq"].t[:], in0=E["kk"].t[:], in1=E["kk"].t[:], op=ALU.mult), [E["kk"]], [E["sq"]])
                    V_(lambda e: e.tensor_reduce(out=sm.t[:, 0:8], in_=E["sq"].t[:, :].rearrange("p (h d) -> p h d", d=64), axis=AX.X, op=ALU.add),
                       [E["sq"]], [sm])
                    A_(lambda e: e.activation(out=sm.t[:, 0:8], in_=sm.t[:, 0:8], func=AF.Sqrt), [sm], [sm])
                    V_(lambda e: e.tensor_scalar(out=sm.t[:, 0:8], in0=sm.t[:, 0:8], scalar1=1e-12, scalar2=None, op0=ALU.max), [sm], [sm])
                    V_(lambda e: e.reciprocal(out=sm.t[:, 0:8], in_=sm.t[:, 0:8]), [sm], [sm])
                    V_(lambda e: e.tensor_tensor(out=E["kkn"].t[:, :].rearrange("p (h d) -> p h d", d=64),
                                                 in0=E["kk"].t[:, :].rearrange("p (h d) -> p h d", d=64),
                                                 in1=sm.t[:, 0:8].unsqueeze(2).to_broadcast([128, 8, 64]), op=ALU.mult), [E["kk"], sm], [E["kkn"]])
                    A_(lambda e: e.mul(out=E["as"].t[:], in_=E["kkn"].t[:], mul=-1.0), [E["kkn"]], [E["as"]])
                    pstore("a", E["as"])
                    G_(lambda e: e.tensor_tensor(out=E["bs"].t[:], in0=E["kkn"].t[:], in1=E["a"].t[:], op=ALU.mult), [E["kkn"], E["a"]], [E["bs"]])
                    pstore("b", E["bs"])
                    V_(lambda e: e.scalar_tensor_tensor(out=E["t"].t[:], in0=E["a"].t[:], scalar=-1.0, in1=bcc["k_a"].t[:],
                                                        op0=ALU.add, op1=ALU.mult), [E["a"], bcc["k_a"]], [E["t"]])
                    V_(lambda e: e.scalar_tensor_tensor(out=E["k2"].t[:], in0=E["t"].t[:], scalar=1.0, in1=E["k"].t[:],
                                                        op0=ALU.add, op1=ALU.mult), [E["t"], E["k"]], [E["k2"]])
                    pstore("k", E["k2"])
                    G_(lambda e: e.tensor_tensor(out=E["u"].t[:], in0=E["r"].t[:], in1=E["k2"].t[:], op=ALU.mult), [E["r"], E["k2"]], [E["u"]])
                    G_(lambda e: e.tensor_tensor(out=E["u"].t[:], in0=E["u"].t[:], in1=bcc["r_k"].t[:], op=ALU.mult), [E["u"], bcc["r_k"]], [E["u"]])
                    hb = n0 // 64
                    V_(lambda e: e.tensor_reduce(out=bon.t[:, hb:hb + 8], in_=E["u"].t[:, :].rearrange("p (h d) -> p h d", d=64),
                                                 axis=AX.X, op=ALU.add), [E["u"]], [bon])
                s.dma("act", self.scr["bon"][t0:t0 + 128, :], bon.t[:], reads=[bon])
                s.op("dve", lambda e: e.tensor_copy(out=hT.t[:, :, 0:1], in_=hT.t[:, :, 128:129]), reads=[hT], writes=[hT])

    def phase_rw_scan(self):
        c, s = self.c, self.s
        D, H = c.D, c.H
        NPR = H // 2
        RW = D // 2
        NCH = (RW + 511) // 512
        CW = min(512, RW)
        with contextlib.ExitStack() as es:
            F = self.sb(es, "sc_F", [128, 64 * 128])
            rows = {nm: [self.sb(es, "sc_row_%s%d" % (nm, i), [128, RW]) for i in range(2)] for nm in ("a", "w", "b", "k", "r")}
            vt = self.sb(es, "sc_vt", [128, D])
            vcol = [self.sb(es, "sc_vcol%d" % i, [128, NPR, 128]) for i in range(2)]
            ybuf = [self.sb(es, "sc_ybuf%d" % i, [128, NPR, 128]) for i in range(2)]
            yt = [self.sb(es, "sc_yt%d" % i, [128, D]) for i in range(2)]
            St = self.sb(es, "sc_S", [128, RW])
            T = [self.sb(es, "sc_T%d" % i, [128, RW]) for i in range(2)]
            sa = self.sb(es, "sc_sa", [128, NPR])
            s.dma("sp", F.t[:], self.ins["fsel"], writes=[F])
            s.op("dve", lambda e: e.memset(St.t[:], 0.0), writes=[St])
            v3 = lambda ap: ap.rearrange("p (j k) -> p j k", k=64)
            slot = 0
            for bi in range(c.S // 128):
                t0 = bi * 128
                vc, yb = vcol[bi % 2], ybuf[bi % 2]
                s.dma("sp", vt.t[:], self.scr["v"][t0:t0 + 128, :], writes=[vt])
                for j in range(NPR):
                    ps = self.PS[j % 2]
                    s.op("pe", lambda e, j=j, ps=ps: e.transpose(ps.t[:, 0:128], vt.t[:, j * 128:(j + 1) * 128], self.ident.t[:]),
                         reads=[vt, self.ident], writes=[ps])
                    s.op("act", lambda e, j=j, ps=ps: e.copy(out=vc.t[:, j, :], in_=ps.t[:, 0:128]), reads=[ps], writes=[vc])
                for half in range(2):
                    hb = (bi * 2 + half) % 2
                    tb = t0 + half * 64
                    for nm in ("a", "w", "b", "k", "r"):
                        s.dma("sp", rows[nm][hb].t[:], self.scr[nm][tb:tb + 64, :].rearrange("t (hh x) -> (t hh) x", hh=2),
                              writes=[rows[nm][hb]])
                    for tt in range(64):
                        tl = half * 64 + tt
                        sel = F.t[:, tt * 128:(tt + 1) * 128]

                        def bcast(nm):
                            nonlocal slot
                            pw = self.PSW[slot % 4]
                            slot += 1
                            for ch in range(NCH):
                                s.op("pe", lambda e, ch=ch, pw=pw: e.matmul(pw.t[:, ch * 512:ch * 512 + CW], lhsT=sel,
                                                                            rhs=rows[nm][hb].t[:, ch * 512:ch * 512 + CW],
                                                                            start=True, stop=True),
                                     reads=[F, rows[nm][hb]], writes=[pw])
                            return pw
                        V_ = lambda fn, rd, wr: s.op("dve", fn, reads=rd, writes=wr)
                        pa = bcast("a")
                        pw_ = bcast("w")
                        pb = bcast("b")
                        pk = bcast("k")
                        T0, T1 = T[0], T[1]
                        V_(lambda e: e.tensor_tensor(out=T0.t[:], in0=St.t[:], in1=pa.t[:, 0:RW], op=ALU.mult), [St, pa], [T0])
                        V_(lambda e: e.tensor_reduce(out=sa.t[:, :], in_=v3(T0.t[:, :]), axis=AX.X, op=ALU.add), [T0], [sa])
                        V_(lambda e: e.tensor_tensor(out=St.t[:], in0=St.t[:], in1=pw_.t[:, 0:RW], op=ALU.mult), [St, pw_], [St])
                        V_(lambda e: e.tensor_tensor(out=v3(T1.t[:, :]), in0=v3(pb.t[:, 0:RW]),
                                                     in1=sa.t[:, :].unsqueeze(2).to_broadcast([128, NPR, 64]), op=ALU.mult), [pb, sa], [T1])
                        V_(lambda e: e.tensor_tensor(out=St.t[:], in0=St.t[:], in1=T1.t[:], op=ALU.add), [St, T1], [St])
                        V_(lambda e: e.tensor_tensor(out=v3(T0.t[:, :]), in0=v3(pk.t[:, 0:RW]),
                                                     in1=vc.t[:, :, tl].unsqueeze(2).to_broadcast([128, NPR, 64]), op=ALU.mult), [pk, vc], [T0])
                        pr = bcast("r")
                        V_(lambda e: e.tensor_tensor(out=St.t[:], in0=St.t[:], in1=T0.t[:], op=ALU.add), [St, T0], [St])
                        V_(lambda e: e.tensor_tensor(out=T1.t[:], in0=St.t[:], in1=pr.t[:, 0:RW], op=ALU.mult), [St, pr], [T1])
                        V_(lambda e: e.tensor_reduce(out=yb.t[:, :, tl], in_=v3(T1.t[:, :]), axis=AX.X, op=ALU.add), [T1], [yb])
                yo = yt[bi % 2]
                for j in range(NPR):
                    ps = self.PS[j % 2]
                    s.op("pe", lambda e, j=j, ps=ps: e.transpose(ps.t[:, 0:128], yb.t[:, j, :], self.ident.t[:]),
                         reads=[yb, self.ident], writes=[ps])
                    s.op("act", lambda e, j=j, ps=ps: e.copy(out=yo.t[:, j * 128:(j + 1) * 128], in_=ps.t[:, 0:128]), reads=[ps], writes=[yo])
                s.dma("act", self.scr["y"][t0:t0 + 128, :], yo.t[:], reads=[yo])

    def phase_rw_out(self, idx, src, dst):
        c, s = self.c, self.s
        D, KC, H = c.D, c.KC, c.H
        NT3 = min(256, c.S)
        NS = NT3 // 128
        I = self.ins
        h3 = lambda ap: ap.rearrange("p (h d) -> p h d", d=64)
        bc3 = lambda ap: ap.unsqueeze(2).to_broadcast([128, H, 64])
        with contextlib.ExitStack() as es:
            xs = [self.sb(es, "ro_x%d" % i, [128, D]) for i in range(NS)]
            y_ = self.sb(es, "ro_y", [128, D])
            v_ = self.sb(es, "ro_v", [128, D])
            g_ = self.sb(es, "ro_g", [128, D])
            tm = self.sb(es, "ro_tm", [128, D])
            lng = self.sb(es, "ro_lng", [128, D])
            lnb = self.sb(es, "ro_lnb", [128, D])
            bon = self.sb(es, "ro_bon", [128, H])
            st_ = self.sb(es, "ro_st", [128, 2 * H])
            zT = self.sb(es, "ro_zT", [128, KC, NT3], BF16)
            wst = [self.sb(es, "ro_wst%d" % i, [128, KC * 256]) for i in range(2)]
            wbf = [self.sb(es, "ro_wbf%d" % i, [128, KC * 512], BF16) for i in range(2)]
            s.dma("sp", lng.t[:], I["rw_lnx_g_bc"][idx], writes=[lng])
            s.dma("sp", lnb.t[:], I["rw_lnx_b_bc"][idx], writes=[lnb])
            V_ = lambda fn, rd, wr: s.op("dve", fn, reads=rd, writes=wr)
            G_ = lambda fn, rd, wr: s.op("pool", fn, reads=rd, writes=wr)
            wi = 0
            for st in range(c.S // NT3):
                for sub in range(NS):
                    t0 = st * NT3 + sub * 128
                    s.dma("sp", xs[sub].t[:], src[t0:t0 + 128, :], writes=[xs[sub]])
                    s.dma("sp", y_.t[:], self.scr["y"][t0:t0 + 128, :], writes=[y_])
                    s.dma("sp", v_.t[:], self.scr["v"][t0:t0 + 128, :], writes=[v_])
                    s.dma("sp", g_.t[:], self.scr["g"][t0:t0 + 128, :], writes=[g_])
                    s.dma("sp", bon.t[:], self.scr["bon"][t0:t0 + 128, :], writes=[bon])
                    V_(lambda e: e.tensor_reduce(out=st_.t[:, 0:H], in_=h3(y_.t[:, :]), axis=AX.X, op=ALU.add), [y_], [st_])
                    V_(lambda e: e.tensor_scalar(out=st_.t[:, 0:H], in0=st_.t[:, 0:H], scalar1=1.0 / 64, scalar2=None, op0=ALU.mult), [st_], [st_])
                    V_(lambda e: e.tensor_tensor(out=h3(y_.t[:, :]), in0=h3(y_.t[:, :]), in1=bc3(st_.t[:, 0:H]), op=ALU.subtract), [y_, st_], [y_])
                    G_(lambda e: e.tensor_tensor(out=tm.t[:], in0=y_.t[:], in1=y_.t[:], op=ALU.mult), [y_], [tm])
                    V_(lambda e: e.tensor_reduce(out=st_.t[:, H:2 * H], in_=h3(tm.t[:, :]), axis=AX.X, op=ALU.add), [tm], [st_])
                    V_(lambda e: e.tensor_scalar(out=st_.t[:, H:2 * H], in0=st_.t[:, H:2 * H], scalar1=1.0 / 64, scalar2=64e-5,
                                                 op0=ALU.mult, op1=ALU.add), [st_], [st_])
                    s.op("act", lambda e: e.activation(out=st_.t[:, H:2 * H], in_=st_.t[:, H:2 * H], func=AF.Sqrt), reads=[st_], writes=[st_])
                    V_(lambda e: e.reciprocal(out=st_.t[:, H:2 * H], in_=st_.t[:, H:2 * H]), [st_], [st_])
                    V_(lambda e: e.tensor_tensor(out=h3(y_.t[:, :]), in0=h3(y_.t[:, :]), in1=bc3(st_.t[:, H:2 * H]), op=ALU.mult), [y_, st_], [y_])
                    G_(lambda e: e.tensor_tensor(out=y_.t[:], in0=y_.t[:], in1=lng.t[:], op=ALU.mult), [y_, lng], [y_])
                    G_(lambda e: e.tensor_tensor(out=y_.t[:], in0=y_.t[:], in1=lnb.t[:], op=ALU.add), [y_, lnb], [y_])
                    V_(lambda e: e.tensor_tensor(out=h3(tm.t[:, :]), in0=h3(v_.t[:, :]), in1=bc3(bon.t[:, 0:H]), op=ALU.mult), [v_, bon], [tm])
                    G_(lambda e: e.tensor_tensor(out=y_.t[:], in0=y_.t[:], in1=tm.t[:], op=ALU.add), [y_, tm], [y_])
                    V_(lambda e: e.tensor_tensor(out=tm.t[:], in0=y_.t[:], in1=g_.t[:], op=ALU.mult), [y_, g_], [tm])
                    self.transpose_to(tm, D, [zT], sub * 128, self.PS[4:8])
                q = 0
                for n0 in range(0, D, 512):
                    wb = wbf[wi % 2]
                    wi += 1
                    self.load_w(I["rw_w_o"][idx][:, n0:n0 + 512], D, 512, wst, wb)
                    for sub in range(NS):
                        ps = self.PS[q % 4]
                        q += 1
                        for kc in range(KC):
                            s.op("pe", lambda e, kc=kc, sub=sub, ps=ps, wb=wb: e.matmul(
                                ps.t[:, :], lhsT=zT.t[:, kc, sub * 128:(sub + 1) * 128], rhs=wb.t[:, kc * 512:(kc + 1) * 512],
                                start=(kc == 0), stop=(kc == KC - 1)), reads=[zT, wb], writes=[ps])
                        V_(lambda e, ps=ps, n0=n0: e.tensor_tensor(out=tm.t[:, 0:512], in0=ps.t[:, :], in1=self.G.t[:, n0:n0 + 512], op=ALU.mult),
                           [ps, self.G], [tm])
                        V_(lambda e, sub=sub, n0=n0: e.tensor_tensor(out=xs[sub].t[:, n0:n0 + 512], in0=xs[sub].t[:, n0:n0 + 512],
                                                                     in1=tm.t[:, 0:512], op=ALU.add), [tm, xs[sub]], [xs[sub]])
                for sub in range(NS):
                    t0 = st * NT3 + sub * 128
                    s.dma("act", dst[t0:t0 + 128, :], xs[sub].t[:], reads=[xs[sub]])

    def phase_moe(self, layer, src, dst):
        c, s = self.c, self.s
        D, KC, DE = c.D, c.KC, c.DE
        NT = 1024 if c.S % 1024 == 0 else c.NT
        NSUB = NT // 128
        HW = min(512, NT)
        NH = NT // HW
        fk = DE // 128
        WSZ = max(KC * DE, fk * D)
        with contextlib.ExitStack() as es:
            xs = [self.sb(es, "mo_x%d" % i, [128, D]) for i in range(NSUB)]
            h = self.sb(es, "mo_h", [128, D])
            ss = self.sb(es, "mo_ss", [128, 2])
            hT = self.sb(es, "mo_hT", [128, KC, NT], BF16)
            hT32 = self.sb(es, "mo_hT32", [128, KC * 128])
            wr = self.sb(es, "mo_wr", [128, KC, 36])
            brb = self.sb(es, "mo_brb", [128, 36])
            wst = [self.sb(es, "mo_wst0", [128, KC * 128]), hT32]
            assert KC * 128 >= D
            wbf = [self.sb(es, "mo_wbf%d" % i, [128, WSZ], BF16) for i in range(3)]
            hid = [self.sb(es, "mo_hid%d" % i, [128, fk, NT], BF16) for i in range(1)]
            coef = [self.sb(es, "mo_coef%d" % i, [128, 32]) for i in range(NSUB)]
            rt = self.sb(es, "mo_rt", [128, 160])
            s.dma("sp", wr.t[:], self.ins["moe_w_r"][layer].rearrange("(kc p) n -> p kc n", p=128), writes=[wr])
            s.dma("sp", brb.t[:], self.ins["moe_b_r_bc"][layer], writes=[brb])
            wi = 0
            for st in range(c.S // NT):
                for sub in range(NSUB):
                    t0 = st * NT + sub * 128
                    x_ = xs[sub]
                    s.dma("sp", x_.t[:], src[t0:t0 + 128, :], writes=[x_])
                    self.norm_tile(x_, h, ss)
                    self.transpose_to(h, D, [hT], sub * 128, self.PS[4:8])
                    self._transpose32(h, hT32)
                    self.router(hT32, wr, brb, rt, coef[sub])
                for e_ in range(c.NE):
                    wg, wu, wd = wbf[wi % 3], wbf[(wi + 1) % 3], wbf[(wi + 2) % 3]
                    self.load_w(self.ins["moe_w_gate"][layer, e_], D, DE, wst, wg)
                    self.load_w(self.ins["moe_w_up"][layer, e_], D, DE, wst, wu)
                    self.load_w(self.ins["moe_w_down"][layer, e_], DE, D, wst, wd, scale_bc=self.G)
                    wi += 3
                    hd = hid[0]
                    gi = 0
                    for fc in range(fk):
                        for hf in range(NH):
                            pg, pu = self.PS[(gi % 2) * 2], self.PS[(gi % 2) * 2 + 1]
                            sgv = h.t[:, (gi % max(1, min(2, D // HW))) * HW:(gi % max(1, min(2, D // HW)) + 1) * HW]
                            gi += 1
                            tsl = slice(hf * HW, (hf + 1) * HW)
                            for kc in range(KC):
                                s.op("pe", lambda e, kc=kc, fc=fc, pg=pg, wg=wg, tsl=tsl: e.matmul(
                                    pg.t[:, 0:HW], lhsT=wg.t[:, kc * DE + fc * 128: kc * DE + (fc + 1) * 128], rhs=hT.t[:, kc, tsl],
                                    start=(kc == 0), stop=(kc == KC - 1)), reads=[wg, hT], writes=[pg])
                            for kc in range(KC):
                                s.op("pe", lambda e, kc=kc, fc=fc, pu=pu, wu=wu, tsl=tsl: e.matmul(
                                    pu.t[:, 0:HW], lhsT=wu.t[:, kc * DE + fc * 128: kc * DE + (fc + 1) * 128], rhs=hT.t[:, kc, tsl],
                                    start=(kc == 0), stop=(kc == KC - 1)), reads=[wu, hT], writes=[pu])
                            s.op("act", lambda e, pg=pg, sgv=sgv: e.activation(out=sgv, in_=pg.t[:, 0:HW], func=AF.Silu),
                                 reads=[pg], writes=[h])
                            s.op("dve", lambda e, pu=pu, sgv=sgv, fc=fc, hd=hd, tsl=tsl: e.tensor_tensor(
                                out=hd.t[:, fc, tsl], in0=pu.t[:, 0:HW], in1=sgv, op=ALU.mult),
                                reads=[pu, h], writes=[hd])
                    q = 0
                    for sub in range(NSUB):
                        for n0 in range(0, D, 512):
                            ps = self.PS[4 + q % 4]
                            q += 1
                            for fc in range(fk):
                                s.op("pe", lambda e, fc=fc, sub=sub, n0=n0, ps=ps, wd=wd, hd=hd: e.matmul(
                                    ps.t[:, :], lhsT=hd.t[:, fc, sub * 128:(sub + 1) * 128],
                                    rhs=wd.t[:, fc * D + n0: fc * D + n0 + 512],
                                    start=(fc == 0), stop=(fc == fk - 1)), reads=[hd, wd], writes=[ps])
                            s.op("dve", lambda e, sub=sub, n0=n0, ps=ps, e_=e_: e.scalar_tensor_tensor(
                                out=xs[sub].t[:, n0:n0 + 512], in0=ps.t[:, :], scalar=coef[sub].t[:, e_:e_ + 1],
                                in1=xs[sub].t[:, n0:n0 + 512], op0=ALU.mult, op1=ALU.add),
                                reads=[ps, coef[sub], xs[sub]], writes=[xs[sub]])
                for sub in range(NSUB):
                    t0 = st * NT + sub * 128
                    s.dma("act", dst[t0:t0 + 128, :], xs[sub].t[:], reads=[xs[sub]])

    def _transpose32(self, h, hT32):
        s = self.s
        nk = self.c.KC
        g = 0
        for k0 in range(0, nk, 4):
            kn = min(4, nk - k0)
            ps = self.PS[4 + g % 4]
            g += 1
            for j in range(kn):
                s.op("pe", lambda e, j=j, k0=k0, ps=ps: e.transpose(ps.t[:, j * 128:(j + 1) * 128],
                                                                    h.t[:, (k0 + j) * 128:(k0 + j + 1) * 128], self.ident.t[:]),
                     reads=[h, self.ident], writes=[ps])
            s.op("dve", lambda e, ps=ps, k0=k0, kn=kn: e.tensor_copy(
                out=hT32.t[:, k0 * 128:(k0 + kn) * 128], in_=ps.t[:, 0:kn * 128]),
                reads=[ps], writes=[hT32])

    def router(self, hT32, wr, brb, rt, coef):
        c, s = self.c, self.s
        KC = c.KC
        ps = self.PS[3]
        for kc in range(KC):
            s.op("pe", lambda e, kc=kc: e.matmul(ps.t[:, 0:36], lhsT=hT32.t[:, kc * 128:(kc + 1) * 128], rhs=wr.t[:, kc, :],
                                                  start=(kc == 0), stop=(kc == KC - 1)), reads=[hT32, wr], writes=[ps])
        R = rt.t
        V = lambda fn, rd=(rt,), wr_=(rt,): s.op("dve", fn, reads=list(rd), writes=list(wr_))
        V(lambda e: e.tensor_tensor(out=R[:, 0:36], in0=ps.t[:, 0:36], in1=brb.t[:, :], op=ALU.add), rd=(ps, brb))
        V(lambda e: e.tensor_reduce(out=R[:, 36:37], in_=R[:, 0:4], axis=AX.X, op=ALU.max))
        V(lambda e: e.tensor_scalar(out=R[:, 38:42], in0=R[:, 0:4], scalar1=R[:, 36:37], scalar2=None, op0=ALU.is_equal))
        V(lambda e: e.tensor_scalar(out=R[:, 42:46], in0=R[:, 0:4], scalar1=R[:, 36:37], scalar2=None, op0=ALU.subtract))
        s.op("act", lambda e: e.activation(out=R[:, 42:46], in_=R[:, 42:46], func=AF.Exp, accum_out=R[:, 37:38]),
             reads=[rt], writes=[rt])
        V(lambda e: e.reciprocal(out=R[:, 116:117], in_=R[:, 37:38]))
        V(lambda e: e.tensor_tensor(out=R[:, 48:80].rearrange("p (g j) -> p g j", g=4),
                                    in0=R[:, 4:36].rearrange("p (g j) -> p g j", g=4),
                                    in1=R[:, 38:42].rearrange("p (g o) -> p g o", o=1).to_broadcast([128, 4, 8])
                                    if hasattr(R[:, 38:42], "to_broadcast") else None, op=ALU.mult)) if False else None
        for g in range(4):
            if g == 0:
                V(lambda e: e.tensor_scalar(out=R[:, 80:88], in0=R[:, 4:12], scalar1=R[:, 38:39], scalar2=None, op0=ALU.mult))
            else:
                V(lambda e, g=g: e.scalar_tensor_tensor(out=R[:, 80:88], in0=R[:, 4 + 8 * g:12 + 8 * g], scalar=R[:, 38 + g:39 + g],
                                                        in1=R[:, 80:88], op0=ALU.mult, op1=ALU.add))
        V(lambda e: e.tensor_reduce(out=R[:, 88:89], in_=R[:, 80:88], axis=AX.X, op=ALU.max))
        V(lambda e: e.tensor_scalar(out=R[:, 90:98], in0=R[:, 80:88], scalar1=R[:, 88:89], scalar2=None, op0=ALU.is_equal))
        V(lambda e: e.scalar_tensor_tensor(out=R[:, 98:106], in0=R[:, 90:98], scalar=-1e30, in1=R[:, 80:88],
                                           op0=ALU.mult, op1=ALU.add))
        V(lambda e: e.tensor_reduce(out=R[:, 89:90], in_=R[:, 98:106], axis=AX.X, op=ALU.max))
        V(lambda e: e.tensor_scalar(out=R[:, 106:114], in0=R[:, 98:106], scalar1=R[:, 89:90], scalar2=None, op0=ALU.is_equal))
        V(lambda e: e.tensor_tensor(out=R[:, 114:115], in0=R[:, 88:89], in1=R[:, 89:90], op=ALU.subtract))
        s.op("act", lambda e: e.activation(out=R[:, 114:115], in_=R[:, 114:115], func=AF.Sigmoid), reads=[rt], writes=[rt])
        V(lambda e: e.tensor_scalar(out=R[:, 115:116], in0=R[:, 114:115], scalar1=-1.0, scalar2=1.0, op0=ALU.mult, op1=ALU.add))
        V(lambda e: e.tensor_tensor(out=R[:, 114:116], in0=R[:, 114:116], in1=R[:, 116:117].to_broadcast([128, 2])
                                    if False else R[:, 116:117], op=ALU.mult)) if False else None
        V(lambda e: e.tensor_scalar(out=R[:, 114:116], in0=R[:, 114:116], scalar1=R[:, 116:117], scalar2=None, op0=ALU.mult))
        V(lambda e: e.tensor_scalar(out=R[:, 120:128], in0=R[:, 90:98], scalar1=R[:, 114:115], scalar2=None, op0=ALU.mult))
        V(lambda e: e.scalar_tensor_tensor(out=R[:, 120:128], in0=R[:, 106:114], scalar=R[:, 115:116], in1=R[:, 120:128],
                                           op0=ALU.mult, op1=ALU.add))
        for g in range(4):
            s.op("dve", lambda e, g=g: e.tensor_scalar(out=coef.t[:, 8 * g:8 * g + 8], in0=R[:, 120:128], scalar1=R[:, 38 + g:39 + g],
                                                       scalar2=None, op0=ALU.mult), reads=[rt], writes=[coef])


def _t5_bucket(dist):
    n = np.maximum(dist, 0)
    nf = np.maximum(n, 1).astype(np.float32)
    large = 16 + (np.log(nf / 16) / math.log(128 / 16) * 16).astype(np.int32)
    large = np.minimum(large, 31)
    return np.where(n < 16, n, large)


def _bands():
    out = np.zeros((4, 3, 128, 128), np.float32)
    t = np.arange(128)[:, None]
    tp = np.arange(128)[None, :]
    for gi, w in enumerate((2, 4, 8, 16)):
        inw = ((tp - t) >= 0) & ((tp - t) <= w - 1)
        out[gi, 0] = np.where(inw, 1.0 / w, 0.0) - (t == tp)
        out[gi, 1] = np.where((tp + 128 - t) <= w - 1, 1.0 / w, 0.0)
        out[gi, 2] = np.where(inw, 1.0 / np.minimum(tp + 1, w), 0.0) - (t == tp)
    return out


def _biasT(rel_bias):
    j = np.arange(128)[:, None, None]
    blk = np.arange(2)[None, :, None]
    i = np.arange(128)[None, None, :]
    b = _t5_bucket(128 + i - (blk * 128 + j))
    t = np.asarray(rel_bias, np.float32)[b]
    return np.ascontiguousarray(np.transpose(t, (0, 3, 1, 2)).reshape(128, -1, 256))


def _maskT():
    j = np.arange(128)[:, None, None]
    blk = np.arange(2)[None, :, None]
    i = np.arange(128)[None, None, :]
    jj = blk * 128 + j
    return np.ascontiguousarray(((jj > i) & (jj <= i + 128)).astype(np.float32).reshape(128, 256))


def make_in_maps(cfg, inp):
    c = cfg
    D, KC = c.D, c.KC
    f = lambda a: np.ascontiguousarray(np.asarray(a, dtype=np.float32))
    bc = lambda a: f(np.broadcast_to(np.asarray(a)[..., None, :], a.shape[:-1] + (128, a.shape[-1])))
    L2 = c.DEPTH * 2
    shared = {
        "norm_g_bc": bc(inp["norm_g"].reshape(L2, D)),
        "ada_w": f(inp["ada_w"].reshape(L2, D, 3 * D)),
        "ada_b": f(inp["ada_b"].reshape(L2, 3 * D)),
        "ident": np.eye(128, dtype=np.float32),
        "ones": np.ones((128, 128), np.float32),
        "pool_w": f(inp["pool_w"]) if c.NP else np.zeros((1, 4, c.PG, c.PG), np.float32),
        "pool_scale_bc": bc(inp["pool_scale"]) if c.NP else np.zeros((1, 128, D), np.float32),
        "bands": _bands(),
        "attn_w_in": f(inp["attn_w_in"]),
        "attn_w_o": f(inp["attn_w_o"]),
        "attn_gq_bc": bc(inp["attn_q_gain"]),
        "attn_gk_bc": bc(inp["attn_k_gain"]),
        "attn_sinks_bc": bc(inp["attn_sinks"]),
        "biasT": _biasT(inp["rel_bias"]),
        "maskT": _maskT(),
        "rw_w_rkv": f(inp["rw_w_rkv"]), "rw_w1": f(inp["rw_w1"]), "rw_a1": f(inp["rw_a1"]), "rw_g1": f(inp["rw_g1"]),
        "rw_w2": f(inp["rw_w2"]), "rw_a2": f(inp["rw_a2"]), "rw_g2": f(inp["rw_g2"]), "rw_w_o": f(inp["rw_w_o"]),
        "rw_muT": f(np.transpose(np.asarray(inp["rw_mu"]).reshape(-1, 6, KC, 128), (0, 3, 1, 2))),
        "rw_w0_bc": bc(inp["rw_w0"]), "rw_a0_bc": bc(inp["rw_a0"]), "rw_k_k_bc": bc(inp["rw_k_k"]), "rw_k_a_bc": bc(inp["rw_k_a"]),
        "rw_r_k_bc": bc(np.asarray(inp["rw_r_k"]).reshape(-1, D)), "rw_lnx_g_bc": bc(inp["rw_lnx_g"]), "rw_lnx_b_bc": bc(inp["rw_lnx_b"]),
        "fsel": np.ascontiguousarray((np.arange(64 * 128)[None, :] // 64 == np.arange(128)[:, None]).astype(np.float32)),
        "moe_w_r": f(np.concatenate([inp["moe_w_grp"], inp["moe_w_exp"]], axis=-1)),
        "moe_b_r_bc": bc(np.concatenate([inp["moe_b_grp"], inp["moe_b_exp"]], axis=-1)),
        "moe_w_gate": f(inp["moe_w_gate"]),
        "moe_w_up": f(inp["moe_w_up"]),
        "moe_w_down": f(inp["moe_w_down"]),
    }
    maps = []
    for b in range(c.B):
        m = dict(shared)
        m["x"] = f(inp["x"][b])
        m["cT"] = f(np.asarray(inp["c"][b]).reshape(KC, 128).T)
        maps.append(m)
    return maps


def kernel(**inputs):
    cfg = Cfg()
    inp = {k: np.asarray(v) for k, v in inputs.items()}
    nc = Builder(cfg).build()
    maps = make_in_maps(cfg, inp)
    res = run_bass_kernel_spmd(nc, maps, core_ids=list(range(cfg.B)))
    return np.stack([r["y"] for r in res.results], axis=0).astype(np.float32)
```
